# Optimizing a Trainium2 kernel written in Bass

```python
import jax, jax.numpy as jnp
from jax import lax
import numpy as np

D_MODEL = 1024
BATCH = 16
SEQ = 4096
DEPTH = 4

MIX_WIDTH = D_MODEL
HGRN_WIDTH = MIX_WIDTH // 2
HGRN_HEADS = 4
HGRN_HEAD_DIM = HGRN_WIDTH // HGRN_HEADS
GLA_VAL = MIX_WIDTH - HGRN_WIDTH
GLA_HEADS = 4
GLA_KEY = GLA_VAL // 2
GLA_DK = GLA_KEY // GLA_HEADS
GLA_DV = GLA_VAL // GLA_HEADS
GLA_RANK = 16
GLA_GATE_NORM = 16.0
IN_SIZES = (HGRN_WIDTH, HGRN_WIDTH, HGRN_WIDTH, HGRN_WIDTH,
            GLA_KEY, GLA_KEY, GLA_VAL, GLA_VAL, GLA_RANK)
IN_DIM = sum(IN_SIZES)
CHUNK = 64
D_FF = 2816
CONV_W = 3
N_MOD = 6
EPS = 1e-6

kernel_name = "hybrid_hgrn2_gla_convffn_adaln"


def rms_norm(x, g):
    xf = x.astype(jnp.float32)
    y = xf * lax.rsqrt(jnp.mean(xf * xf, axis=-1, keepdims=True) + EPS)
    return (y * g.astype(jnp.float32)).astype(x.dtype)


def chunk_gated_linear_attention(q, k, v, log_a):
    B, S, H, DK = q.shape
    DV = v.shape[-1]
    N = S // CHUNK

    def to_chunks(t):
        return t.astype(jnp.float32).reshape(B, N, CHUNK, H, t.shape[-1]).transpose(0, 3, 1, 2, 4)

    q, k, v, g = to_chunks(q), to_chunks(k), to_chunks(v), to_chunks(log_a)
    b = jnp.cumsum(g, axis=3)
    b_last = b[:, :, :, -1:, :]
    ref = b[:, :, :, CHUNK // 2:CHUNK // 2 + 1, :]
    scores = jnp.einsum('bhncd,bhnsd->bhncs', q * jnp.exp(b - ref), k * jnp.exp(ref - b))
    causal = jnp.tril(jnp.ones((CHUNK, CHUNK), dtype=bool))
    scores = jnp.where(causal, scores, 0.0)
    o = jnp.einsum('bhncs,bhnse->bhnce', scores, v)
    u = jnp.einsum('bhncd,bhnce->nbhde', k * jnp.exp(b_last - b), v)
    decay = jnp.exp(b_last[:, :, :, 0, :]).transpose(2, 0, 1, 3)

    def step(state, inp):
        a_n, u_n = inp
        return a_n[..., None] * state + u_n, state

    s0 = jnp.zeros((B, H, DK, DV), jnp.float32)
    _, s_prev = lax.scan(step, s0, (decay, u))
    o = o + jnp.einsum('bhncd,nbhde->bhnce', q * jnp.exp(b), s_prev)
    return o.transpose(0, 2, 3, 1, 4).reshape(B, S, H, DV)


def gated_head_norm(o, gate, g):
    B, S = o.shape[:2]
    o = rms_norm(o, g).reshape(B, S, -1)
    return o * jax.nn.silu(gate.astype(jnp.float32))


def causal_dwconv(u, w, bias):
    S = u.shape[1]
    up = jnp.pad(u, ((0, 0), (CONV_W - 1, 0), (0, 0)))
    y = bias
    for j in range(CONV_W):
        y = y + up[:, j:j + S, :] * w[j]
    return y


def setup_inputs(seed: int = 0) -> dict:
    key = jax.random.key(seed)
    ks = jax.random.split(key, 20)
    f32 = jnp.float32
    nrm = lambda k, shape, s: jax.random.normal(k, shape, f32) * s
    L, D = DEPTH, D_MODEL
    return {
        "x": nrm(ks[0], (BATCH, SEQ, D), 1.0),
        "c": nrm(ks[1], (BATCH, D), 1.0),
        "ln1_g": 1.0 + nrm(ks[2], (L, D), 0.02),
        "ln2_g": 1.0 + nrm(ks[3], (L, D), 0.02),
        "w_ada": nrm(ks[4], (L, D, N_MOD * D), 0.5 * D ** -0.5),
        "b_ada": nrm(ks[5], (L, N_MOD * D), 0.01),
        "w_in": nrm(ks[6], (L, D, IN_DIM), D ** -0.5),
        "lb_params": nrm(ks[7], (L, HGRN_WIDTH), 0.1),
        "w_gk": nrm(ks[8], (L, GLA_RANK, GLA_KEY), GLA_RANK ** -0.5),
        "b_gk": nrm(ks[9], (L, GLA_KEY), 0.01),
        "gn_a": 1.0 + nrm(ks[10], (L, HGRN_HEAD_DIM), 0.02),
        "gn_b": 1.0 + nrm(ks[11], (L, GLA_DV), 0.02),
        "w_out": nrm(ks[12], (L, MIX_WIDTH, D), MIX_WIDTH ** -0.5),
        "w_up": nrm(ks[13], (L, D, 2 * D_FF), D ** -0.5),
        "conv_w": nrm(ks[14], (L, CONV_W, 2 * D_FF), CONV_W ** -0.5),
        "conv_b": nrm(ks[15], (L, 2 * D_FF), 0.01),
        "w_down": nrm(ks[16], (L, D_FF, D), D_FF ** -0.5),
        "lnf_g": 1.0 + nrm(ks[17], (D,), 0.02),
    }


def reference(x, c, ln1_g, ln2_g, w_ada, b_ada, w_in, lb_params, w_gk, b_gk,
              gn_a, gn_b, w_out, w_up, conv_w, conv_b, w_down, lnf_g):
    B, S, _ = x.shape
    split_idx = np.cumsum(IN_SIZES)[:-1].tolist()
    sm = jax.nn.softmax(lb_params.astype(jnp.float32), axis=0)
    lower_bounds = jnp.cumsum(sm, axis=0) - sm[0]
    cond = jax.nn.silu(c)
    for l in range(DEPTH):
        mod = jnp.einsum('bd,de->be', cond, w_ada[l]) + b_ada[l]
        sh1, sc1, gt1, sh2, sc2, gt2 = jnp.split(mod[:, None, :], N_MOD, axis=-1)

        h = rms_norm(x, ln1_g[l]) * (1.0 + sc1) + sh1
        p = jnp.einsum('bsd,de->bse', h, w_in[l])
        qa, fa, ia, ga, qb, kb, vb, gb, rb = jnp.split(p, split_idx, axis=-1)

        fa = fa.astype(jnp.float32)
        lb = lower_bounds[l]
        log_f = jnp.log(lb + (1.0 - lb) * jax.nn.sigmoid(fa))
        k_a = (1.0 - lb) * jax.nn.sigmoid(-fa)
        hs = (B, S, HGRN_HEADS, HGRN_HEAD_DIM)
        o_a = chunk_gated_linear_attention(qa.reshape(hs), k_a.reshape(hs), ia.reshape(hs), log_f.reshape(hs))
        o_a = gated_head_norm(o_a, ga, gn_a[l])

        gk = jnp.einsum('bsr,rk->bsk', rb.astype(jnp.float32), w_gk[l].astype(jnp.float32)) + b_gk[l]
        log_alpha = jax.nn.log_sigmoid(gk) / GLA_GATE_NORM
        ks_ = (B, S, GLA_HEADS, GLA_DK)
        q_b = qb.astype(jnp.float32) * GLA_DK ** -0.5
        o_b = chunk_gated_linear_attention(q_b.reshape(ks_), kb.reshape(ks_),
                                           vb.reshape(B, S, GLA_HEADS, GLA_DV), log_alpha.reshape(ks_))
        o_b = gated_head_norm(o_b, gb, gn_b[l])

        o = jnp.concatenate([o_a, o_b], axis=-1).astype(x.dtype)
        x = x + gt1 * jnp.einsum('bse,ed->bsd', o, w_out[l])

        h = rms_norm(x, ln2_g[l]) * (1.0 + sc2) + sh2
        u = jnp.einsum('bsd,df->bsf', h, w_up[l])
        u = causal_dwconv(u, conv_w[l], conv_b[l])
        a, v = jnp.split(u, 2, axis=-1)
        x = x + gt2 * jnp.einsum('bsf,fd->bsd', jax.nn.silu(a) * v, w_down[l])
    return rms_norm(x, lnf_g)
```

```python
import math
from contextlib import ExitStack

import numpy as np
import concourse.bass as bass
import concourse.mybir as mybir
from concourse.bass_utils import run_bass_kernel_spmd

F32 = mybir.dt.float32
BF16 = mybir.dt.bfloat16
AF = mybir.ActivationFunctionType
ALU = mybir.AluOpType

D = 1024
KC = 8
T = 512
NBLK = 4
NCH = 8
L_TOTAL = 4
IN_DIM = 3600
DFF = 2816
NFF = 22
EPS = 1e-6
NSLOT = 6
NPAGES = 51
QSHIFT = 20.0


class Prog:
    CE = ('pe', 'act', 'dve', 'pool')

    def __init__(self, nc):
        self.nc = nc
        self.streams = {e: [] for e in ('pe', 'act', 'dve', 'pool', 'sp')}
        self.cnt = {}
        self.known = {e: {} for e in self.streams}
        self.bufs = {}
        self.semkeys = []

    def _sem(self, key):
        if key not in self.cnt:
            self.cnt[key] = 0
            self.semkeys.append(key)
        return key

    def _need(self, eng, clock, waits):
        if clock is None:
            return
        sk, val = clock
        if sk == eng and eng == 'pe':
            return
        if self.known[eng].get(sk, 0) >= val:
            return
        waits[sk] = max(waits.get(sk, 0), val)

    def op(self, eng, fn, reads=(), writes=(), dma=None):
        waits = {}
        for k in reads:
            b = self.bufs.get(k)
            if b:
                self._need(eng, b[0], waits)
        for k in writes:
            b = self.bufs.get(k)
            if b:
                self._need(eng, b[0], waits)
                for sk, v in b[1].items():
                    self._need(eng, (sk, v), waits)
        for sk, v in waits.items():
            self.known[eng][sk] = v
        if dma is not None:
            sk = self._sem(('dma', dma))
            self.cnt[sk] += 16
        else:
            sk = self._sem(eng)
            self.cnt[sk] += 1
        clock = (sk, self.cnt[sk])
        for k in reads:
            b = self.bufs.setdefault(k, [None, {}])
            b[1][sk] = max(b[1].get(sk, 0), clock[1])
        for k in writes:
            self.bufs[k] = [clock, {}]
        self.streams[eng].append((list(waits.items()), fn, sk, 16 if dma is not None else 1))
        return clock

    def wait_all(self, eng, keys):
        waits = {}
        for k in keys:
            b = self.bufs.get(k)
            if b:
                self._need(eng, b[0], waits)
        self.streams[eng].append((list(waits.items()), None, None, 0))

    def emit(self):
        nc = self.nc
        with ExitStack() as es:
            sems = {}
            for i, sk in enumerate(self.semkeys):
                sems[sk] = es.enter_context(nc.semaphore("s%d" % i))
            block = es.enter_context(nc.Block())

            def run(stream_name):
                def body(engine):
                    for waits, fn, sk, inc in self.streams[stream_name]:
                        for wk, wv in waits:
                            engine.wait_ge(sems[wk], wv)
                        if fn is not None:
                            fn(engine).then_inc(sems[sk], inc)
                return body
            block.tensor(run('pe'))
            block.scalar(run('act'))
            block.vector(run('dve'))
            block.gpsimd(run('pool'))
            block.sync(run('sp'))


def build_program(n_seq, seq_len, layers, first, final_norm):
    nc = bass.Bass("TRN2", target_bir_lowering=False)
    n_tiles = seq_len // T
    dt_in = lambda name, shape: nc.dram_tensor(name, shape, F32, kind="ExternalInput").ap()
    x_d = dt_in("x", [n_seq, seq_len, D])
    c_d = dt_in("c", [n_seq, D])
    ln1_d = dt_in("ln1_g", [L_TOTAL, D])
    ln2_d = dt_in("ln2_g", [L_TOTAL, D])
    wada_d = dt_in("w_ada", [L_TOTAL, D, 6 * D])
    bada_d = dt_in("b_ada", [L_TOTAL, 6 * D])
    win_d = dt_in("w_in", [L_TOTAL, D, IN_DIM])
    lbp_d = dt_in("lb_params", [L_TOTAL, 512])
    wgk_d = dt_in("w_gk", [L_TOTAL, 16, 256])
    bgk_d = dt_in("b_gk", [L_TOTAL, 256])
    gna_d = dt_in("gn_a", [L_TOTAL, 128])
    gnb_d = dt_in("gn_b", [L_TOTAL, 128])
    wout_d = dt_in("w_out", [L_TOTAL, D, D])
    wup_d = dt_in("w_up", [L_TOTAL, D, 2 * DFF])
    cw_d = dt_in("conv_w", [L_TOTAL, 3, 2 * DFF])
    cb_d = dt_in("conv_b", [L_TOTAL, 2 * DFF])
    wdn_d = dt_in("w_down", [L_TOTAL, DFF, D])
    lnf_d = dt_in("lnf_g", [D])
    ident_d = dt_in("ident", [128, 128])
    cmask_d = dt_in("cmask", [128, 512])
    rmask_d = dt_in("rmask", [128, 512])
    y_d = nc.dram_tensor("y", [n_seq, seq_len, D], F32, kind="ExternalOutput").ap()

    es = ExitStack()
    sb = lambda name, shape, dt: es.enter_context(nc.sbuf_tensor(name, shape, dt))
    X = sb("X", [128, KC, T], F32)
    H = sb("H", [128, KC, T], BF16)
    POOL = sb("POOL", [128, NPAGES, 512], F32)
    POOLB = POOL[:].bitcast(BF16)
    S = sb("S", [128, L_TOTAL, 6, 128], F32)
    HALO = sb("HALO", [128, L_TOTAL, 44, 2], F32)
    RING = sb("RING", [128, NSLOT, KC, 512], BF16)
    DEC = sb("DEC", [128, 6, 8], F32)
    IDF = sb("IDF", [128, 128], F32)
    IDB = sb("IDB", [128, 128], BF16)
    ONESB = sb("ONESB", [128, 128], BF16)
    CMASK = sb("CMASK", [128, 512], F32)
    RMASK = sb("RMASK", [128, 512], F32)
    G1 = sb("G1", [128, L_TOTAL * 8], F32)
    G2 = sb("G2", [128, L_TOTAL * 8], F32)
    LNF = sb("LNF", [128, 8], F32)
    NBGK = sb("NBGK", [128, L_TOTAL * 2], F32)
    GNA = sb("GNA", [128, L_TOTAL], F32)
    GNB = sb("GNB", [128, L_TOTAL], F32)
    CW = sb("CW", [128, L_TOTAL * 3 * 44], F32)
    CB = sb("CB", [128, L_TOTAL * 44], F32)
    LBP = sb("LBP", [128, L_TOTAL * 4], F32)
    OML = sb("OML", [128, L_TOTAL * 4], F32)
    BADA = sb("BADA", [128, L_TOTAL * 48], F32)
    CT = sb("CT", [128, n_seq * 8], F32)
    MOD = sb("MOD", [128, L_TOTAL, 48, n_seq], F32)
    GS1 = sb("GS1", [128, L_TOTAL, 8, n_seq], F32)
    GS2 = sb("GS2", [128, L_TOTAL, 8, n_seq], F32)
    WGK = sb("WGK", [16, L_TOTAL, 256], F32)
    CONST = sb("CONST", [128, 8], F32)
    SM = sb("SM", [128, 64], F32)
    ROWS = sb("ROWS", [128, 128], F32)
    HM = sb("HM", [128, 2], F32)
    CORR = sb("CORR", [128, 44, 2], F32)
    CORT = sb("CORT", [128, 44], F32)
    PS = es.enter_context(nc.psum_tensor("PS", [128, 8, 512], F32))
    PSB = PS[:].bitcast(BF16)

    P = Prog(nc)
    bank_ctr = [0]
    ROT = [list(range(8))]

    def nb():
        r = ROT[0]
        b = r[bank_ctr[0] % len(r)]
        bank_ctr[0] += 1
        return b

    def run_tasks(tasks):
        tasks = [iter(t) for t in tasks]
        while tasks:
            for t in list(tasks):
                try:
                    next(t)
                except StopIteration:
                    tasks.remove(t)

    pk = lambda i: ('pg', i)
    pks = lambda i0, n: [('pg', i) for i in range(i0, i0 + n)]

    def b16(p0, c, n=1):
        page, half = p0 + c // 2, c % 2
        return POOLB[:, page, half * 512:half * 512 + 512 * n]

    P_QA, P_QB, P_KB, P_KA, P_SB16 = 0, 2, 3, 4, 4
    P_GT, P_OG = 10, 10
    P_RSTD, P_LNV, P_TMP0, P_TMP1 = 10, 11, 12, 13
    P_LF, P_BC, P_D1, P_D4, P_E0, P_E1 = 10, 11, 12, 13, 14, 15
    P_QT, P_SQ, P_KT, P_QH, P_KHT, P_KHM = 16, 16, 20, 23, 27, 28
    P_VA, P_VB, P_SGA, P_SGB, P_SCT = 34, 36, 38, 40, 42
    P_TMPS, P_OSB, P_SQH, P_RB = 46, 47, 48, 49
    P_XT = 16
    P_G, P_CV = 16, 0

    eps_ap = CONST[:, 0:1]
    one_ap = CONST[:, 1:2]
    ln8_ap = CONST[:, 2:3]

    P.op('dve', lambda e: e.memset(CONST[:, 0:1], EPS), writes=['CONST'])
    P.op('dve', lambda e: e.memset(CONST[:, 1:2], 1.0), writes=['CONST'])
    P.op('dve', lambda e: e.memset(CONST[:, 2:3], math.log(0.125)), writes=['CONST'])
    P.op('dve', lambda e: e.memset(CONST[:, 3:4], 0.0), writes=['CONST'])
    P.op('dve', lambda e: e.memset(CONST[:, 4:5], -QSHIFT), writes=['CONST'])
    P.op('dve', lambda e: e.memset(CONST[:, 5:6], math.log(0.125) - QSHIFT), writes=['CONST'])
    P.op('dve', lambda e: e.memset(ONESB[:], 1.0), writes=['ONESB'])
    P.op('dve', lambda e: e.memset(HM[:], 0.0), writes=['HM'])
    P.op('dve', lambda e: e.memset(HM[0:64, 0:1], 1.0), writes=['HM'])
    P.op('dve', lambda e: e.memset(HM[64:128, 1:2], 1.0), writes=['HM'])
    P.op('dve', lambda e: e.memset(POOL[:, 0:25, :], 0.0), writes=pks(0, 25))
    P.op('dve', lambda e: e.memset(POOL[:, 25:NPAGES, :], 0.0), writes=pks(25, NPAGES - 25))
    P.op('sp', lambda e: e.dma_start(out=IDF[:], in_=ident_d), writes=['IDF'], dma='c0')
    P.op('sp', lambda e: e.dma_start(out=CMASK[:], in_=cmask_d), writes=['CMASK'], dma='c1')
    P.op('sp', lambda e: e.dma_start(out=RMASK[:], in_=rmask_d), writes=['RMASK'], dma='c2')
    P.op('sp', lambda e: e.dma_start(out=WGK[:], in_=wgk_d.rearrange("l r k -> r l k")), writes=['WGK'], dma='c3')
    P.op('act', lambda e: e.activation(out=IDB[:], in_=IDF[:], func=AF.Copy), reads=['IDF'], writes=['IDB'])

    def load_rows_T(dst, dst_key, src_rows, R):
        P.op('sp', lambda e: e.dma_start(out=ROWS[0:R, :], in_=src_rows), writes=['ROWS'], dma='rows')
        b = nb()
        P.op('pe', lambda e: e.transpose(out=PS[:, b, 0:R], in_=ROWS[0:R, :], identity=IDF[0:R, 0:R]),
             reads=['ROWS', 'IDF'], writes=[('ps', b)])
        P.op('act', lambda e: e.activation(out=dst, in_=PS[:, b, 0:R], func=AF.Copy),
             reads=[('ps', b)], writes=[dst_key])

    def load_param(dst_tile, key, src2d, total_rows):
        r0 = 0
        while r0 < total_rows:
            R = min(128, total_rows - r0)
            load_rows_T(dst_tile[:, r0:r0 + R], key, src2d[r0:r0 + R, :], R)
            r0 += R

    load_param(G1, 'G1', ln1_d.rearrange("l (c p) -> (l c) p", p=128), L_TOTAL * 8)
    load_param(G2, 'G2', ln2_d.rearrange("l (c p) -> (l c) p", p=128), L_TOTAL * 8)
    load_param(LNF, 'LNF', lnf_d.rearrange("(c p) -> c p", p=128), 8)
    load_param(NBGK, 'NBGK', bgk_d.rearrange("l (c p) -> (l c) p", p=128), L_TOTAL * 2)
    load_param(GNA, 'GNA', gna_d, L_TOTAL)
    load_param(GNB, 'GNB', gnb_d, L_TOTAL)
    load_param(CW, 'CW', cw_d.rearrange("l j (m p) -> (l j m) p", p=128), L_TOTAL * 3 * 44)
    load_param(CB, 'CB', cb_d.rearrange("l (m p) -> (l m) p", p=128), L_TOTAL * 44)
    load_param(LBP, 'LBP', lbp_d.rearrange("l (c p) -> (l c) p", p=128), L_TOTAL * 4)
    load_param(BADA, 'BADA', bada_d.rearrange("l (c p) -> (l c) p", p=128), L_TOTAL * 48)
    load_param(CT, 'CT', c_d.rearrange("s (c p) -> (s c) p", p=128), n_seq * 8)
    P.op('dve', lambda e: e.tensor_scalar(out=NBGK[:], in0=NBGK[:], scalar1=-1.0, scalar2=None, op0=ALU.mult),
         reads=['NBGK'], writes=['NBGK'])
    P.op('act', lambda e: e.activation(out=CT[:], in_=CT[:], func=AF.Silu), reads=['CT'], writes=['CT'])
    lb4 = LBP[:].rearrange("p (l c) -> p l c", c=4)
    mx, ex, sm_, rs_ = SM[:, 0:4], SM[:, 4:20].rearrange("p (l c) -> p l c", c=4), SM[:, 20:24], SM[:, 24:28]
    P.op('dve', lambda e: e.tensor_tensor(out=mx, in0=lb4[:, 0, :], in1=lb4[:, 1, :], op=ALU.max), reads=['LBP'], writes=['SM'])
    P.op('dve', lambda e: e.tensor_tensor(out=mx, in0=mx, in1=lb4[:, 2, :], op=ALU.max), reads=['LBP', 'SM'], writes=['SM'])
    P.op('dve', lambda e: e.tensor_tensor(out=mx, in0=mx, in1=lb4[:, 3, :], op=ALU.max), reads=['LBP', 'SM'], writes=['SM'])
    for l in range(4):
        P.op('dve', lambda e, l=l: e.tensor_tensor(out=ex[:, l, :], in0=lb4[:, l, :], in1=mx, op=ALU.subtract),
             reads=['LBP', 'SM'], writes=['SM'])
    P.op('act', lambda e: e.activation(out=SM[:, 4:20], in_=SM[:, 4:20], func=AF.Exp), reads=['SM'], writes=['SM'])
    P.op('dve', lambda e: e.tensor_tensor(out=sm_, in0=ex[:, 0, :], in1=ex[:, 1, :], op=ALU.add), reads=['SM'], writes=['SM'])
    P.op('dve', lambda e: e.tensor_tensor(out=sm_, in0=sm_, in1=ex[:, 2, :], op=ALU.add), reads=['SM'], writes=['SM'])
    P.op('dve', lambda e: e.tensor_tensor(out=sm_, in0=sm_, in1=ex[:, 3, :], op=ALU.add), reads=['SM'], writes=['SM'])
    P.op('dve', lambda e: e.reciprocal(out=rs_, in_=sm_), reads=['SM'], writes=['SM'])
    oml4 = OML[:].rearrange("p (l c) -> p l c", c=4)
    P.op('dve', lambda e: e.memset(OML[:], 1.0), writes=['OML'])
    for l in range(1, 4):
        P.op('dve', lambda e, l=l: e.tensor_tensor(out=ex[:, l, :], in0=ex[:, l, :], in1=rs_, op=ALU.mult),
             reads=['SM'], writes=['SM'])
        P.op('dve', lambda e, l=l: e.tensor_tensor(out=oml4[:, l, :], in0=oml4[:, l - 1, :], in1=ex[:, l, :], op=ALU.subtract),
             reads=['SM', 'OML'], writes=['OML'])

    ct3 = CT[:].rearrange("p (s c) -> p s c", c=8)
    wa_bufs = [(POOL[:, 0:12, :], pks(0, 12)), (POOL[:, 12:24, :], pks(12, 12))]
    wa_i = 0
    for l in layers:
        b = nb()
        for q in range(8):
            buf, bkeys = wa_bufs[wa_i % 2]
            wa_i += 1
            bufv = buf.rearrange("p a n -> p (a n)").rearrange("p (k n) -> p k n", k=8)
            src = wada_d[l].rearrange("(k p) n -> p k n", p=128)[:, :, q * 768:(q + 1) * 768]
            P.op('sp', lambda e, bufv=bufv, src=src: e.dma_start(out=bufv, in_=src), writes=bkeys, dma='wa%d' % (wa_i % 2))
            for j6 in range(6):
                jc = q * 6 + j6
                for kc in range(8):
                    P.op('pe', lambda e, bufv=bufv, j6=j6, jc=jc, kc=kc, b=b: e.matmul(
                        PS[:, b, jc * n_seq:(jc + 1) * n_seq], lhsT=bufv[:, kc, j6 * 128:(j6 + 1) * 128],
                        rhs=ct3[:, :, kc], start=(kc == 0), stop=(kc == 7)),
                        reads=bkeys + ['CT'], writes=[('ps', b)])
        bada3 = BADA[:, l * 48:(l + 1) * 48]
        P.op('dve', lambda e, l=l, b=b, bada3=bada3: e.tensor_tensor(
            out=MOD[:, l, :, :], in0=PS[:, b, 0:48 * n_seq].rearrange("p (j s) -> p j s", s=n_seq),
            in1=bada3.unsqueeze(2).to_broadcast([128, 48, n_seq]), op=ALU.add),
            reads=[('ps', b), 'BADA'], writes=['MOD'])
        for (GS, Gp, joff) in ((GS1, G1, 1), (GS2, G2, 4)):
            P.op('dve', lambda e, l=l, GS=GS, joff=joff: e.tensor_scalar(
                out=GS[:, l, :, :], in0=MOD[:, l, joff * 8:(joff + 1) * 8, :], scalar1=1.0, scalar2=None, op0=ALU.add),
                reads=['MOD'], writes=['GS'])
            P.op('dve', lambda e, l=l, GS=GS, Gp=Gp: e.tensor_tensor(
                out=GS[:, l, :, :], in0=GS[:, l, :, :],
                in1=Gp[:, l * 8:(l + 1) * 8].unsqueeze(2).to_broadcast([128, 8, n_seq]), op=ALU.mult),
                reads=['GS', 'G1', 'G2'], writes=['GS'])
    P.op('dve', lambda e: e.memset(POOL[:, 0:24, :], 0.0), writes=pks(0, 24))

    wloads = []

    def plan_weights():
        for s in range(n_seq):
            for t in range(n_tiles):
                for l in layers:
                    wv = win_d[l].rearrange("(k p) n -> p k n", p=128)
                    for ci in (1, 0, 7, 4, 2, 5, 3, 6):
                        if ci == 7:
                            wloads.append([(lambda sl: RING[:, sl, :, 0:16], wv[:, :, 3584:3600])])
                        else:
                            wloads.append([(lambda sl: RING[:, sl, :, :], wv[:, :, ci * 512:(ci + 1) * 512])])
                    ov = wout_d[l].rearrange("(k p) n -> p k n", p=128)
                    for ci in range(2):
                        wloads.append([(lambda sl: RING[:, sl, :, :], ov[:, :, ci * 512:(ci + 1) * 512])])
                    uv = wup_d[l].rearrange("(k p) n -> p k n", p=128)
                    for i in range(11):
                        wloads.append([
                            (lambda sl: RING[:, sl, :, 0:256], uv[:, :, 256 * i:256 * i + 256]),
                            (lambda sl: RING[:, sl, :, 256:512], uv[:, :, DFF + 256 * i:DFF + 256 * i + 256])])
                    dv = wdn_d[l].rearrange("(k p) n -> p k n", p=128)
                    for hf in range(2):
                        for g3 in range(3):
                            k0 = g3 * 8
                            k1 = min(22, k0 + 8)
                            wloads.append([(lambda sl, k0=k0, k1=k1: RING[:, sl, 0:k1 - k0, :],
                                            dv[:, k0:k1, hf * 512:(hf + 1) * 512])])
    plan_weights()
    wstate = {'issued': 0, 'consumed': 0}

    def issue_load():
        i = wstate['issued']
        if i >= len(wloads):
            return
        sl = i % NSLOT
        for (dstf, src) in wloads[i]:
            dst = dstf(sl)
            P.op('pool', lambda e, dst=dst, src=src: e.dma_start(out=dst, in_=src), writes=[('W', sl)], dma='w%d' % sl)
        wstate['issued'] += 1

    def next_w():
        i = wstate['consumed']
        wstate['consumed'] += 1
        return i % NSLOT

    def release_w():
        issue_load()

    for _ in range(NSLOT):
        issue_load()

    Hkeys = [('H', k) for k in range(8)]
    Xkeys = [('X', k) for k in range(8)]

    def norm_mod(src_keys_fn, l, s, GS, shoff):
        for dc in range(8):
            P.op('act', lambda e, dc=dc: e.activation(out=b16(P_SQ, dc), in_=X[:, dc, :], func=AF.Square),
                 reads=[('X', dc)], writes=[pk(P_SQ + dc // 2)])
        b = nb()
        for dc in range(8):
            P.op('pe', lambda e, dc=dc, b=b: e.matmul(PS[:, b, :], lhsT=ONESB[:], rhs=b16(P_SQ, dc),
                                                     start=(dc == 0), stop=(dc == 7)),
                 reads=[pk(P_SQ + dc // 2), 'ONESB'], writes=[('ps', b)])
        P.op('act', lambda e, b=b: e.activation(out=POOL[:, P_LNV, :], in_=PS[:, b, :], func=AF.Ln, scale=1.0 / D, bias=eps_ap),
             reads=[('ps', b), 'CONST'], writes=[pk(P_LNV)])
        P.op('act', lambda e: e.activation(out=POOL[:, P_RSTD, :], in_=POOL[:, P_LNV, :], func=AF.Exp, scale=-0.5),
             reads=[pk(P_LNV)], writes=[pk(P_RSTD)])
        for dc in range(8):
            tp = P_TMP0 + (dc % 2)
            P.op('dve', lambda e, dc=dc, tp=tp: e.tensor_tensor(out=POOL[:, tp, :], in0=X[:, dc, :], in1=POOL[:, P_RSTD, :], op=ALU.mult),
                 reads=[('X', dc), pk(P_RSTD)], writes=[pk(tp)])
            P.op('act', lambda e, dc=dc, tp=tp: e.activation(out=H[:, dc, :], in_=POOL[:, tp, :], func=AF.Identity,
                                                            scale=GS[:, l, dc, s:s + 1], bias=MOD[:, l, shoff * 8 + dc, s:s + 1]),
                 reads=[pk(tp), 'GS', 'MOD'], writes=[('H', dc)])

    def mm_fm(sl, col0, ncols_chunks, evac):
        for ci in range(ncols_chunks):
            b = nb()
            for kc in range(8):
                P.op('pe', lambda e, ci=ci, kc=kc, b=b: e.matmul(
                    PS[:, b, :], lhsT=RING[:, sl, kc, col0 + ci * 128:col0 + (ci + 1) * 128], rhs=H[:, kc, :],
                    start=(kc == 0), stop=(kc == 7)),
                    reads=[('W', sl), ('H', kc)], writes=[('ps', b)])
            evac(ci, b)
            yield

    def mm_tm(sl, dst_page):
        for blk in range(NBLK):
            b = nb()
            for kc in range(8):
                P.op('pe', lambda e, kc=kc, b=b, blk=blk: e.matmul(
                    PS[:, b, :], lhsT=H[:, kc, blk * 128:(blk + 1) * 128], rhs=RING[:, sl, kc, :],
                    start=(kc == 0), stop=(kc == 7)),
                    reads=[('W', sl), ('H', kc)], writes=[('ps', b)])
            P.op('act', lambda e, b=b, blk=blk: e.activation(out=b16(dst_page, blk), in_=PS[:, b, :], func=AF.Copy),
                 reads=[('ps', b)], writes=[pk(dst_page + blk // 2)])
            yield

    TSETS = [dict(LF=10, BC=11, D1=12, D4=13, E=[14, 15, 8, 9]), dict(LF=42, BC=43, D1=44, D4=45, E=[46, 47, 48, 50])]

    def gate_chunk(fc, l, ts):
        T_LF, T_BC, T_D1, T_D4 = ts['LF'], ts['BC'], ts['D1'], ts['D4']
        T_E = ts['E']
        if fc < 4:
            hc = fc
            sigma, qb_ap, qt_ap = 1.0, CONST[:, 3:4], CONST[:, 4:5]
            q_ap, k_ap = b16(P_QA, hc), POOL[:, P_KA + hc, :]
            qk_keys = [pk(P_QA + hc // 2), pk(P_KA + hc)]
            P.op('dve', lambda e: e.tensor_scalar(out=POOL[:, P_KA + hc, :], in0=POOL[:, P_KA + hc, :],
                                                  scalar1=OML[:, l * 4 + hc:l * 4 + hc + 1], scalar2=None, op0=ALU.mult),
                 reads=[pk(P_KA + hc), 'OML'], writes=[pk(P_KA + hc)])
            P.op('act', lambda e: e.activation(out=POOL[:, T_LF, :], in_=POOL[:, P_KA + hc, :], func=AF.Ln, scale=-1.0, bias=one_ap),
                 reads=[pk(P_KA + hc), 'CONST'], writes=[pk(T_LF)])
        else:
            c2 = fc - 4
            sigma, qb_ap, qt_ap = -1.0 / 16.0, ln8_ap, CONST[:, 5:6]
            q_ap, k_ap = b16(P_QB, c2), b16(P_KB, c2)
            qk_keys = [pk(P_QB), pk(P_KB)]
            bg = nb()
            P.op('pe', lambda e: e.matmul(PS[:, bg, :], lhsT=WGK[0:16, l, c2 * 128:(c2 + 1) * 128], rhs=POOL[0:16, P_RB, :],
                                          start=True, stop=True),
                 reads=[pk(P_RB), 'WGK'], writes=[('ps', bg)])
            P.op('act', lambda e: e.activation(out=POOL[:, T_E[0], :], in_=PS[:, bg, :], func=AF.Exp, scale=-1.0,
                                               bias=NBGK[:, l * 2 + c2:l * 2 + c2 + 1]),
                 reads=[('ps', bg), 'NBGK'], writes=[pk(T_E[0])])
            P.op('act', lambda e: e.activation(out=POOL[:, T_LF, :], in_=POOL[:, T_E[0], :], func=AF.Ln, bias=one_ap),
                 reads=[pk(T_E[0]), 'CONST'], writes=[pk(T_LF)])
        yield
        bc3 = POOL[:, T_BC, :].rearrange("p (c t) -> p c t", t=64)
        P.op('dve', lambda e: e.tensor_tensor_scan(out=POOL[:, T_BC, :], data0=RMASK[:], data1=POOL[:, T_LF, :],
                                                   initial=0.0, op0=ALU.mult, op1=ALU.add),
             reads=[pk(T_LF), 'RMASK'], writes=[pk(T_BC)])
        yield
        P.op('dve', lambda e: e.tensor_tensor(out=POOL[:, T_D1, :].rearrange("p (c t) -> p c t", t=64), in0=bc3,
                                              in1=bc3[:, :, 32:33].to_broadcast([128, 8, 64]), op=ALU.subtract),
             reads=[pk(T_BC)], writes=[pk(T_D1)])
        P.op('act', lambda e: e.activation(out=DEC[:, fc, :], in_=POOL[:, T_BC, 63:512:64], func=AF.Exp, scale=sigma),
             reads=[pk(T_BC)], writes=[('DEC', fc)])
        yield
        P.op('dve', lambda e: e.tensor_tensor(out=POOL[:, T_D4, :].rearrange("p (c t) -> p c t", t=64),
                                              in0=bc3[:, :, 63:64].to_broadcast([128, 8, 64]), in1=bc3, op=ALU.subtract),
             reads=[pk(T_BC)], writes=[pk(T_D4)])
        yield
        plan = [
            (T_D1, sigma, qt_ap, 'q', 'QT'),
            (T_D1, -sigma, CONST[:, 3:4], 'k', 'KT'),
            (T_BC, sigma, qb_ap, 'q', 'QH'),
            (T_D4, sigma, CONST[:, 3:4], 'k', 'KHT'),
        ]
        for i, (src, sc, bias, which, kind) in enumerate(plan):
            ep = T_E[i]
            P.op('act', lambda e, src=src, sc=sc, bias=bias, ep=ep: e.activation(
                out=POOL[:, ep, :], in_=POOL[:, src, :], func=AF.Exp, scale=sc, bias=bias),
                reads=[pk(src), 'CONST'], writes=[pk(ep)])
            yield
            src_ap = q_ap if which == 'q' else k_ap
            if kind in ('QT', 'QH'):
                p0 = P_QT if kind == 'QT' else P_QH
                if fc < 4:
                    P.op('dve', lambda e, ep=ep, src_ap=src_ap, p0=p0: e.tensor_tensor(
                        out=b16(p0, fc), in0=src_ap, in1=POOL[:, ep, :], op=ALU.mult),
                        reads=[pk(ep)] + qk_keys, writes=[pk(p0 + fc // 2)])
                else:
                    for hh in range(2):
                        g = (fc - 4) * 2 + hh
                        P.op('dve', lambda e, ep=ep, src_ap=src_ap, p0=p0, g=g, hh=hh: e.scalar_tensor_tensor(
                            out=b16(p0, 4 + g), in0=src_ap, scalar=HM[:, hh:hh + 1], in1=POOL[:, ep, :],
                            op0=ALU.mult, op1=ALU.mult),
                            reads=[pk(ep), 'HM'] + qk_keys, writes=[pk(p0 + (4 + g) // 2)])
            elif kind == 'KT':
                P.op('dve', lambda e, ep=ep, src_ap=src_ap: e.tensor_tensor(
                    out=b16(P_KT, fc), in0=src_ap, in1=POOL[:, ep, :], op=ALU.mult),
                    reads=[pk(ep)] + qk_keys, writes=[pk(P_KT + fc // 2)])
            else:
                kht = b16(P_KHT, fc % 2)
                P.op('dve', lambda e, ep=ep, src_ap=src_ap, kht=kht: e.tensor_tensor(
                    out=kht, in0=src_ap, in1=POOL[:, ep, :], op=ALU.mult),
                    reads=[pk(ep)] + qk_keys, writes=[pk(P_KHT)])
                yield
                b = nb()
                for blk in range(NBLK):
                    P.op('pe', lambda e, b=b, blk=blk, kht=kht: e.transpose(
                        out=PSB[:, b, blk * 128:(blk + 1) * 128], in_=kht[:, blk * 128:(blk + 1) * 128], identity=IDB[:]),
                        reads=[pk(P_KHT), 'IDB'], writes=[('ps', b)])
                khm = POOLB[:, P_KHM + fc, :].rearrange("p (j d) -> p j d", d=128)
                pv = PSB[:, b, 0:512].rearrange("p (k d) -> p k d", d=128)
                P.op('act', lambda e, khm=khm, pv=pv, b=b: e.activation(out=khm[0:64, 0:8:2, :], in_=pv[0:64, :, :], func=AF.Copy),
                     reads=[('ps', b)], writes=[pk(P_KHM + fc)])
                P.op('act', lambda e, khm=khm, pv=pv, b=b: e.activation(out=khm[64:128, 1:8:2, :], in_=pv[64:128, :, :], func=AF.Copy),
                     reads=[('ps', b)], writes=[pk(P_KHM + fc)])
            yield

    def layer_step(l, s, t, first_tile):
        norm_mod(None, l, s, GS1, 0)
        flags = {}

        def ev_qa(ci, b):
            P.op('act', lambda e: e.activation(out=b16(P_QA, ci), in_=PS[:, b, :], func=AF.Copy),
                 reads=[('ps', b)], writes=[pk(P_QA + ci // 2)])

        def ev_fa(ci, b):
            P.op('act', lambda e: e.activation(out=POOL[:, P_KA + ci, :], in_=PS[:, b, :], func=AF.Sigmoid, scale=-1.0),
                 reads=[('ps', b)], writes=[pk(P_KA + ci)])

        def ev_sga(ci, b):
            P.op('act', lambda e: e.activation(out=b16(P_SGA, ci), in_=PS[:, b, :], func=AF.Silu),
                 reads=[('ps', b)], writes=[pk(P_SGA + ci // 2)])

        def ev_sgb(ci, b):
            P.op('act', lambda e: e.activation(out=b16(P_SGB, ci), in_=PS[:, b, :], func=AF.Silu),
                 reads=[('ps', b)], writes=[pk(P_SGB + ci // 2)])

        def ev_qk(ci, b):
            if ci < 2:
                P.op('act', lambda e: e.activation(out=b16(P_QB, ci), in_=PS[:, b, :], func=AF.Copy),
                     reads=[('ps', b)], writes=[pk(P_QB)])
            else:
                P.op('act', lambda e: e.activation(out=b16(P_KB, ci - 2), in_=PS[:, b, :], func=AF.Copy),
                     reads=[('ps', b)], writes=[pk(P_KB)])

        sl = next_w()
        for _ in mm_fm(sl, 0, 4, ev_fa):
            pass
        release_w()
        sl = next_w()
        for _ in mm_fm(sl, 0, 4, ev_qa):
            pass
        release_w()

        def main_task():
            sl = next_w()
            b = nb()
            for kc in range(8):
                P.op('pe', lambda e, kc=kc, b=b, sl=sl: e.matmul(PS[0:16, b, :], lhsT=RING[:, sl, kc, 0:16], rhs=H[:, kc, :],
                                                                start=(kc == 0), stop=(kc == 7)),
                     reads=[('W', sl), ('H', kc)], writes=[('ps', b)])
            P.op('act', lambda e, b=b: e.activation(out=POOL[0:16, P_RB, :], in_=PS[0:16, b, :], func=AF.Copy),
                 reads=[('ps', b)], writes=[pk(P_RB)])
            release_w()
            yield
            sl = next_w()
            yield from mm_fm(sl, 0, 4, ev_qk)
            release_w()
            flags['gla'] = True
            sl = next_w()
            yield from mm_tm(sl, P_VA)
            release_w()
            sl = next_w()
            yield from mm_tm(sl, P_VB)
            release_w()
            sl = next_w()
            yield from mm_fm(sl, 0, 4, ev_sga)
            release_w()
            sl = next_w()
            yield from mm_fm(sl, 0, 4, ev_sgb)
            release_w()

        def gate_task(fcs, ts):
            for fc in fcs:
                if fc >= 4:
                    while not flags.get('gla'):
                        yield
                yield from gate_chunk(fc, l, ts)

        run_tasks([main_task(), gate_task([0, 2, 4], TSETS[0]), gate_task([1, 3, 5], TSETS[1])])

        def head_info(h):
            if h < 4:
                return dict(fc=h, qt=b16(P_QT, h), qtk=pk(P_QT + h // 2), kt=b16(P_KT, h), ktk=pk(P_KT + h // 2),
                            qh=b16(P_QH, h), qhk=pk(P_QH + h // 2), vpage=P_VA, vcol=h * 128,
                            gn=GNA[:, l:l + 1], sg=b16(P_SGA, h), sgk=pk(P_SGA + h // 2))
            g = h - 4
            fc = 4 + g // 2
            return dict(fc=fc, qt=b16(P_QT, 4 + g), qtk=pk(P_QT + (4 + g) // 2), kt=b16(P_KT, fc), ktk=pk(P_KT + fc // 2),
                        qh=b16(P_QH, 4 + g), qhk=pk(P_QH + (4 + g) // 2), vpage=P_VB, vcol=g * 128,
                        gn=GNB[:, l:l + 1], sg=b16(P_SGB, g), sgk=pk(P_SGB + g // 2))

        def scores_task():
            for h in range(8):
                hi = head_info(h)
                b = nb()
                for blk in range(NBLK):
                    P.op('pe', lambda e, hi=hi, b=b, blk=blk: e.matmul(
                        PS[:, b, blk * 128:(blk + 1) * 128], lhsT=hi['kt'][:, blk * 128:(blk + 1) * 128],
                        rhs=hi['qt'][:, blk * 128:(blk + 1) * 128], start=True, stop=True),
                        reads=[hi['ktk'], hi['qtk']], writes=[('ps', b)])
                P.op('dve', lambda e, b=b: e.tensor_scalar(out=POOL[:, P_TMPS, :], in0=PS[:, b, :], scalar1=1e30, scalar2=-1e30,
                                                           op0=ALU.min, op1=ALU.max),
                     reads=[('ps', b)], writes=[pk(P_TMPS)])
                P.op('pool', lambda e, h=h: e.tensor_tensor(out=b16(P_SCT, h), in0=POOL[:, P_TMPS, :], in1=CMASK[:], op=ALU.mult),
                     reads=[pk(P_TMPS), 'CMASK'], writes=[pk(P_SCT + h // 2)])
                yield

        def state_task(fcs, banks):
            for i, fc in enumerate(fcs):
                skey = ('S', l, fc)
                if first_tile:
                    P.op('dve', lambda e, fc=fc: e.memset(S[:, l, fc, :], 0.0), writes=[skey])
                for j in range(NCH):
                    bnk = banks[2 * i + (j % 2)]
                    reg = (j // 2) * 128
                    if fc < 4:
                        P.op('pe', lambda e, fc=fc, j=j, bnk=bnk, reg=reg: e.matmul(
                            PS[:, bnk, reg:reg + 128], lhsT=POOLB[:, P_KHM + fc, j * 128:(j + 1) * 128],
                            rhs=b16(P_VA, j // 2)[:, fc * 128:(fc + 1) * 128], start=True, stop=True),
                            reads=[pk(P_KHM + fc), pk(P_VA + (j // 2) // 2)], writes=[('ps', bnk)])
                    else:
                        for hh in range(2):
                            g = (fc - 4) * 2 + hh
                            P.op('pe', lambda e, fc=fc, j=j, bnk=bnk, reg=reg, hh=hh, g=g: e.matmul(
                                PS[hh * 64:(hh + 1) * 64, bnk, reg:reg + 128],
                                lhsT=POOLB[:, P_KHM + fc, j * 128 + hh * 64:j * 128 + (hh + 1) * 64],
                                rhs=b16(P_VB, j // 2)[:, g * 128:(g + 1) * 128], start=True, stop=True),
                                reads=[pk(P_KHM + fc), pk(P_VB + (j // 2) // 2)], writes=[('ps', bnk)])
                yield
            for j in range(NCH):
                for i, fc in enumerate(fcs):
                    skey = ('S', l, fc)
                    sb16 = POOLB[:, P_SB16 + fc, :].rearrange("p (j e) -> p j e", e=128)
                    bnk = banks[2 * i + (j % 2)]
                    reg = (j // 2) * 128
                    P.op('act', lambda e, fc=fc, j=j, sb16=sb16: e.activation(out=sb16[:, j, :], in_=S[:, l, fc, :], func=AF.Copy),
                         reads=[skey], writes=[pk(P_SB16 + fc)])
                    P.op('dve', lambda e, fc=fc, j=j, bnk=bnk, reg=reg: e.scalar_tensor_tensor(
                        out=S[:, l, fc, :], in0=S[:, l, fc, :], scalar=DEC[:, fc, j:j + 1], in1=PS[:, bnk, reg:reg + 128],
                        op0=ALU.mult, op1=ALU.add),
                        reads=[skey, ('DEC', fc), ('ps', bnk)], writes=[skey])
                yield

        def o_task(heads, tmps=P_TMPS, osb=P_OSB, sqh=0):
            for h in heads:
                hi = head_info(h)
                fc = hi['fc']
                sb16 = POOLB[:, P_SB16 + fc, :].rearrange("p (j e) -> p j e", e=128)
                b = nb()
                for blk in range(NBLK):
                    P.op('pe', lambda e, hi=hi, b=b, blk=blk, h=h: e.matmul(
                        PS[:, b, blk * 128:(blk + 1) * 128], lhsT=b16(hi['vpage'], blk)[:, hi['vcol']:hi['vcol'] + 128],
                        rhs=b16(P_SCT, h)[:, blk * 128:(blk + 1) * 128], start=True, stop=False),
                        reads=[pk(hi['vpage'] + blk // 2), pk(P_SCT + h // 2)], writes=[('ps', b)])
                    for jj in range(2):
                        j = blk * 2 + jj
                        P.op('pe', lambda e, hi=hi, b=b, j=j, sb16=sb16, jj=jj: e.matmul(
                            PS[:, b, j * 64:(j + 1) * 64], lhsT=sb16[:, j, :], rhs=hi['qh'][:, j * 64:(j + 1) * 64],
                            start=False, stop=(jj == 1)),
                            reads=[pk(P_SB16 + fc), hi['qhk']], writes=[('ps', b)])
                yield
                P.op('act', lambda e, b=b: e.activation(out=b16(P_SQH, sqh), in_=PS[:, b, :], func=AF.Square),
                     reads=[('ps', b)], writes=[pk(P_SQH)])
                b2 = nb()
                P.op('pe', lambda e, b2=b2: e.matmul(PS[:, b2, :], lhsT=ONESB[:], rhs=b16(P_SQH, sqh), start=True, stop=True),
                     reads=[pk(P_SQH), 'ONESB'], writes=[('ps', b2)])
                yield
                P.op('act', lambda e, b2=b2: e.activation(out=POOL[:, tmps, :], in_=PS[:, b2, :], func=AF.Ln, scale=1.0 / 128.0, bias=eps_ap),
                     reads=[('ps', b2), 'CONST'], writes=[pk(tmps)])
                yield
                P.op('act', lambda e: e.activation(out=POOL[:, tmps, :], in_=POOL[:, tmps, :], func=AF.Exp, scale=-0.5),
                     reads=[pk(tmps)], writes=[pk(tmps)])
                yield
                P.op('dve', lambda e, hi=hi, b=b: e.scalar_tensor_tensor(out=POOL[:, osb, :], in0=PS[:, b, :], scalar=hi['gn'],
                                                                        in1=POOL[:, tmps, :], op0=ALU.mult, op1=ALU.mult),
                     reads=[('ps', b), pk(tmps), 'GNA', 'GNB'], writes=[pk(osb)])
                yield
                P.op('pool', lambda e, hi=hi, h=h: e.tensor_tensor(out=b16(P_OG, h), in0=POOL[:, osb, :], in1=hi['sg'], op=ALU.mult),
                     reads=[pk(osb), hi['sgk']], writes=[pk(P_OG + h // 2)])
                yield

        ROT[0] = [6, 7]
        run_tasks([scores_task(), state_task([0, 1, 2], [0, 1, 2, 3, 4, 5])])
        run_tasks([state_task([3, 4, 5], [0, 1, 2, 3, 4, 5]), o_task([0, 1, 2])])
        ROT[0] = list(range(8))
        run_tasks([o_task([3, 5, 7]), o_task([4, 6], tmps=14, osb=15, sqh=1)])

        slots = [next_w(), next_w()]
        for n in range(8):
            sl = slots[n // 4]
            b = nb()
            for kc in range(8):
                P.op('pe', lambda e, n=n, kc=kc, b=b, sl=sl: e.matmul(
                    PS[:, b, :], lhsT=RING[:, sl, kc, (n % 4) * 128:(n % 4 + 1) * 128], rhs=b16(P_OG, kc),
                    start=(kc == 0), stop=(kc == 7)),
                    reads=[('W', sl), pk(P_OG + kc // 2)], writes=[('ps', b)])
            P.op('dve', lambda e, n=n, b=b: e.scalar_tensor_tensor(
                out=X[:, n, :], in0=PS[:, b, :], scalar=MOD[:, l, 2 * 8 + n, s:s + 1], in1=X[:, n, :], op0=ALU.mult, op1=ALU.add),
                reads=[('ps', b), ('X', n), 'MOD'], writes=[('X', n)])
            if n == 3:
                release_w()
        release_w()

        norm_mod(None, l, s, GS2, 3)

        hkey = ('HALO', l)
        if first_tile:
            P.op('dve', lambda e: e.memset(HALO[:, l, :, :], 0.0), writes=[hkey])
        W0 = CW[:, (l * 3 + 0) * 44:(l * 3 + 0) * 44 + 44]
        W1 = CW[:, (l * 3 + 1) * 44:(l * 3 + 1) * 44 + 44]
        P.op('dve', lambda e: e.tensor_tensor(out=CORR[:, :, 1], in0=W0, in1=HALO[:, l, :, 1], op=ALU.mult),
             reads=[hkey, 'CW'], writes=['CORR'])
        P.op('dve', lambda e: e.tensor_tensor(out=CORR[:, :, 0], in0=W0, in1=HALO[:, l, :, 0], op=ALU.mult),
             reads=[hkey, 'CW'], writes=['CORR'])
        P.op('dve', lambda e: e.tensor_tensor(out=CORT[:], in0=W1, in1=HALO[:, l, :, 1], op=ALU.mult),
             reads=[hkey, 'CW'], writes=['CORT'])
        P.op('dve', lambda e: e.tensor_tensor(out=CORR[:, :, 0], in0=CORR[:, :, 0], in1=CORT[:], op=ALU.add),
             reads=['CORR', 'CORT'], writes=['CORR'])

        def conv(b, m, ypage):
            w0 = CW[:, (l * 3 + 0) * 44 + m:(l * 3 + 0) * 44 + m + 1]
            w1 = CW[:, (l * 3 + 1) * 44 + m:(l * 3 + 1) * 44 + m + 1]
            w2 = CW[:, (l * 3 + 2) * 44 + m:(l * 3 + 2) * 44 + m + 1]
            cb = CB[:, l * 44 + m:l * 44 + m + 1]
            Y = POOL[:, ypage, :]
            P.op('act', lambda e: e.activation(out=Y, in_=PS[:, b, :], func=AF.Identity, scale=w2, bias=cb),
                 reads=[('ps', b), 'CW', 'CB'], writes=[pk(ypage)])
            P.op('dve', lambda e: e.scalar_tensor_tensor(out=Y[:, 1:512], in0=PS[:, b, 0:511], scalar=w1, in1=Y[:, 1:512],
                                                         op0=ALU.mult, op1=ALU.add),
                 reads=[('ps', b), pk(ypage), 'CW'], writes=[pk(ypage)])
            P.op('dve', lambda e: e.scalar_tensor_tensor(out=Y[:, 2:512], in0=PS[:, b, 0:510], scalar=w0, in1=Y[:, 2:512],
                                                         op0=ALU.mult, op1=ALU.add),
                 reads=[('ps', b), pk(ypage), 'CW'], writes=[pk(ypage)])
            P.op('dve', lambda e: e.tensor_tensor(out=Y[:, 0:2], in0=Y[:, 0:2], in1=CORR[:, m, :], op=ALU.add),
                 reads=['CORR', pk(ypage)], writes=[pk(ypage)])
            P.op('act', lambda e: e.activation(out=HALO[:, l, m, :], in_=PS[:, b, 510:512], func=AF.Copy),
                 reads=[('ps', b)], writes=[hkey])

        cvi = 0
        for i in range(11):
            sl = next_w()
            for q in range(2):
                m = 2 * i + q
                ya, yv, sa = P_CV + 3 * (cvi % 2), P_CV + 3 * (cvi % 2) + 1, P_CV + 3 * (cvi % 2) + 2
                cvi += 1
                ba = nb()
                for kc in range(8):
                    P.op('pe', lambda e, kc=kc, ba=ba, q=q, sl=sl: e.matmul(
                        PS[:, ba, :], lhsT=RING[:, sl, kc, q * 128:(q + 1) * 128], rhs=H[:, kc, :],
                        start=(kc == 0), stop=(kc == 7)),
                        reads=[('W', sl), ('H', kc)], writes=[('ps', ba)])
                bv = nb()
                for kc in range(8):
                    P.op('pe', lambda e, kc=kc, bv=bv, q=q, sl=sl: e.matmul(
                        PS[:, bv, :], lhsT=RING[:, sl, kc, 256 + q * 128:256 + (q + 1) * 128], rhs=H[:, kc, :],
                        start=(kc == 0), stop=(kc == 7)),
                        reads=[('W', sl), ('H', kc)], writes=[('ps', bv)])
                conv(ba, m, ya)
                conv(bv, NFF + m, yv)
                P.op('act', lambda e, ya=ya, sa=sa: e.activation(out=POOL[:, sa, :], in_=POOL[:, ya, :], func=AF.Silu),
                     reads=[pk(ya)], writes=[pk(sa)])
                P.op('pool', lambda e, sa=sa, yv=yv, m=m: e.tensor_tensor(out=b16(P_G, m), in0=POOL[:, sa, :], in1=POOL[:, yv, :], op=ALU.mult),
                     reads=[pk(sa), pk(yv)], writes=[pk(P_G + m // 2)])
            release_w()

        for hf in range(2):
            slots = [next_w(), next_w(), next_w()]
            for n4 in range(4):
                n = hf * 4 + n4
                b = nb()
                for kf in range(NFF):
                    sl = slots[kf // 8]
                    P.op('pe', lambda e, kf=kf, b=b, sl=sl, n4=n4: e.matmul(
                        PS[:, b, :], lhsT=RING[:, sl, kf % 8, n4 * 128:(n4 + 1) * 128], rhs=b16(P_G, kf),
                        start=(kf == 0), stop=(kf == NFF - 1)),
                        reads=[('W', sl), pk(P_G + kf // 2)], writes=[('ps', b)])
                P.op('dve', lambda e, n=n, b=b: e.scalar_tensor_tensor(
                    out=X[:, n, :], in0=PS[:, b, :], scalar=MOD[:, l, 5 * 8 + n, s:s + 1], in1=X[:, n, :], op0=ALU.mult, op1=ALU.add),
                    reads=[('ps', b), ('X', n), 'MOD'], writes=[('X', n)])
            release_w()
            release_w()
            release_w()

    XT = POOL[:, P_XT:P_XT + 8, :].rearrange("p (b a) n -> p b (a n)", a=2)
    for s in range(n_seq):
        for t in range(n_tiles):
            t0 = t * T
            for blk in range(NBLK):
                P.op('sp', lambda e, blk=blk, s=s, t0=t0: e.dma_start(out=XT[:, blk, :], in_=x_d[s, t0 + blk * 128:t0 + (blk + 1) * 128, :]),
                     writes=pks(P_XT + 2 * blk, 2), dma='xin%d' % blk)
            for dc in range(8):
                b = nb()
                for blk in range(NBLK):
                    P.op('pe', lambda e, b=b, blk=blk, dc=dc: e.transpose(
                        out=PS[:, b, blk * 128:(blk + 1) * 128], in_=XT[:, blk, dc * 128:(dc + 1) * 128], identity=IDF[:]),
                        reads=pks(P_XT + 2 * blk, 2) + ['IDF'], writes=[('ps', b)])
                P.op('act', lambda e, b=b, dc=dc: e.activation(out=X[:, dc, :], in_=PS[:, b, :], func=AF.Copy),
                     reads=[('ps', b)], writes=[('X', dc)])
            for l in layers:
                layer_step(l, s, t, t == 0)
            src_is_X = True
            if final_norm:
                for dc in range(8):
                    P.op('act', lambda e, dc=dc: e.activation(out=b16(P_SQ, dc), in_=X[:, dc, :], func=AF.Square),
                         reads=[('X', dc)], writes=[pk(P_SQ + dc // 2)])
                b = nb()
                for dc in range(8):
                    P.op('pe', lambda e, dc=dc, b=b: e.matmul(PS[:, b, :], lhsT=ONESB[:], rhs=b16(P_SQ, dc),
                                                             start=(dc == 0), stop=(dc == 7)),
                         reads=[pk(P_SQ + dc // 2), 'ONESB'], writes=[('ps', b)])
                P.op('act', lambda e, b=b: e.activation(out=POOL[:, P_LNV, :], in_=PS[:, b, :], func=AF.Ln, scale=1.0 / D, bias=eps_ap),
                     reads=[('ps', b), 'CONST'], writes=[pk(P_LNV)])
                P.op('act', lambda e: e.activation(out=POOL[:, P_RSTD, :], in_=POOL[:, P_LNV, :], func=AF.Exp, scale=-0.5),
                     reads=[pk(P_LNV)], writes=[pk(P_RSTD)])
                for dc in range(8):
                    P.op('dve', lambda e, dc=dc: e.scalar_tensor_tensor(
                        out=X[:, dc, :], in0=X[:, dc, :], scalar=LNF[:, dc:dc + 1], in1=POOL[:, P_RSTD, :], op0=ALU.mult, op1=ALU.mult),
                        reads=[('X', dc), pk(P_RSTD), 'LNF'], writes=[('X', dc)])
            for blk in range(NBLK):
                for half in range(2):
                    b = nb()
                    for d4 in range(4):
                        dc = half * 4 + d4
                        P.op('pe', lambda e, b=b, blk=blk, dc=dc, d4=d4: e.transpose(
                            out=PS[:, b, d4 * 128:(d4 + 1) * 128], in_=X[:, dc, blk * 128:(blk + 1) * 128], identity=IDF[:]),
                            reads=[('X', dc), 'IDF'], writes=[('ps', b)])
                    P.op('act', lambda e, b=b, blk=blk, half=half: e.activation(
                        out=XT[:, blk, half * 512:(half + 1) * 512], in_=PS[:, b, :], func=AF.Copy),
                        reads=[('ps', b)], writes=[pk(P_XT + 2 * blk + half)])
                P.op('sp', lambda e, blk=blk, s=s, t0=t0: e.dma_start(out=y_d[s, t0 + blk * 128:t0 + (blk + 1) * 128, :], in_=XT[:, blk, :]),
                     reads=pks(P_XT + 2 * blk, 2), writes=[('yout', blk)], dma='yout%d' % blk)
    P.wait_all('sp', [('yout', blk) for blk in range(NBLK)])
    P.emit()
    es.close()
    return nc


def _consts():
    ident = np.eye(128, dtype=np.float32)
    p = np.arange(128)[:, None]
    col = np.arange(512)[None, :]
    c = col % 128
    cmask = ((p // 64 == c // 64) & (p % 64 <= c % 64)).astype(np.float32) * np.float32(math.exp(QSHIFT))
    rmask = np.broadcast_to((col % 64 != 0).astype(np.float32), (128, 512)).copy()
    return ident, cmask, rmask


WEIGHT_NAMES = ["ln1_g", "ln2_g", "w_ada", "b_ada", "w_in", "lb_params", "w_gk", "b_gk", "gn_a", "gn_b",
                "w_out", "w_up", "conv_w", "conv_b", "w_down", "lnf_g"]


def run_launch(x, c, weights, layers, final_norm, n_cores):
    B, S_, _ = x.shape
    n_seq = B // n_cores
    nc = build_program(n_seq, S_, layers, True, final_norm)
    ident, cmask, rmask = _consts()
    in_maps = []
    for i in range(n_cores):
        m = {"x": np.ascontiguousarray(x[i * n_seq:(i + 1) * n_seq]),
             "c": np.ascontiguousarray(c[i * n_seq:(i + 1) * n_seq]),
             "ident": ident, "cmask": cmask, "rmask": rmask}
        for k in WEIGHT_NAMES:
            m[k] = weights[k]
        in_maps.append(m)
    res = run_bass_kernel_spmd(nc, in_maps, core_ids=list(range(n_cores)))
    return np.concatenate([r["y"] for r in res.results], axis=0)


FUSED = True


def kernel(**inputs):
    x = np.ascontiguousarray(np.asarray(inputs["x"], dtype=np.float32))
    c = np.ascontiguousarray(np.asarray(inputs["c"], dtype=np.float32))
    weights = {k: np.ascontiguousarray(np.asarray(inputs[k], dtype=np.float32)) for k in WEIGHT_NAMES}
    if FUSED:
        return run_launch(x, c, weights, [0, 1, 2, 3], True, 8)
    for l in range(4):
        x = run_launch(x, c, weights, [l], l == 3, 8)
    return x
```

```python
import math
from contextlib import ExitStack

import numpy as np
import concourse.bass as bass
import concourse.mybir as mybir
from concourse.bass_utils import run_bass_kernel_spmd

F32 = mybir.dt.float32
BF16 = mybir.dt.bfloat16
AF = mybir.ActivationFunctionType
ALU = mybir.AluOpType

D = 1024
KC = 8
T = 512
NBLK = 4
NCH = 8
L_TOTAL = 4
IN_DIM = 3600
DFF = 2816
NFF = 22
EPS = 1e-6
NSLOT = 6
NPAGES = 51
QSHIFT = 20.0


class Prog:
    CE = ('pe', 'act', 'dve', 'pool')

    def __init__(self, nc):
        self.nc = nc
        self.streams = {e: [] for e in ('pe', 'act', 'dve', 'pool', 'sp')}
        self.cnt = {}
        self.known = {e: {} for e in self.streams}
        self.bufs = {}
        self.semkeys = []

    def _sem(self, key):
        if key not in self.cnt:
            self.cnt[key] = 0
            self.semkeys.append(key)
        return key

    def _need(self, eng, clock, waits):
        if clock is None:
            return
        sk, val = clock
        if sk == eng and eng == 'pe':
            return
        if self.known[eng].get(sk, 0) >= val:
            return
        waits[sk] = max(waits.get(sk, 0), val)

    def op(self, eng, fn, reads=(), writes=(), dma=None):
        waits = {}
        for k in reads:
            b = self.bufs.get(k)
            if b:
                self._need(eng, b[0], waits)
        for k in writes:
            b = self.bufs.get(k)
            if b:
                self._need(eng, b[0], waits)
                for sk, v in b[1].items():
                    self._need(eng, (sk, v), waits)
        for sk, v in waits.items():
            self.known[eng][sk] = v
        if dma is not None:
            sk = self._sem(('dma', dma))
            self.cnt[sk] += 16
        else:
            sk = self._sem(eng)
            self.cnt[sk] += 1
        clock = (sk, self.cnt[sk])
        for k in reads:
            b = self.bufs.setdefault(k, [None, {}])
            b[1][sk] = max(b[1].get(sk, 0), clock[1])
        for k in writes:
            self.bufs[k] = [clock, {}]
        self.streams[eng].append((list(waits.items()), fn, sk, 16 if dma is not None else 1))
        return clock

    def wait_all(self, eng, keys):
        waits = {}
        for k in keys:
            b = self.bufs.get(k)
            if b:
                self._need(eng, b[0], waits)
        self.streams[eng].append((list(waits.items()), None, None, 0))

    def emit(self):
        nc = self.nc
        with ExitStack() as es:
            sems = {}
            for i, sk in enumerate(self.semkeys):
                sems[sk] = es.enter_context(nc.semaphore("s%d" % i))
            block = es.enter_context(nc.Block())

            def run(stream_name):
                def body(engine):
                    for waits, fn, sk, inc in self.streams[stream_name]:
                        for wk, wv in waits:
                            engine.wait_ge(sems[wk], wv)
                        if fn is not None:
                            fn(engine).then_inc(sems[sk], inc)
                return body
            block.tensor(run('pe'))
            block.scalar(run('act'))
            block.vector(run('dve'))
            block.gpsimd(run('pool'))
            block.sync(run('sp'))


def build_program(n_seq, seq_len, layers, first, final_norm):
    nc = bass.Bass("TRN2", target_bir_lowering=False)
    n_tiles = seq_len // T
    dt_in = lambda name, shape: nc.dram_tensor(name, shape, F32, kind="ExternalInput").ap()
    x_d = dt_in("x", [n_seq, seq_len, D])
    c_d = dt_in("c", [n_seq, D])
    ln1_d = dt_in("ln1_g", [L_TOTAL, D])
    ln2_d = dt_in("ln2_g", [L_TOTAL, D])
    wada_d = dt_in("w_ada", [L_TOTAL, D, 6 * D])
    bada_d = dt_in("b_ada", [L_TOTAL, 6 * D])
    win_d = dt_in("w_in", [L_TOTAL, D, IN_DIM])
    lbp_d = dt_in("lb_params", [L_TOTAL, 512])
    wgk_d = dt_in("w_gk", [L_TOTAL, 16, 256])
    bgk_d = dt_in("b_gk", [L_TOTAL, 256])
    gna_d = dt_in("gn_a", [L_TOTAL, 128])
    gnb_d = dt_in("gn_b", [L_TOTAL, 128])
    wout_d = dt_in("w_out", [L_TOTAL, D, D])
    wup_d = dt_in("w_up", [L_TOTAL, D, 2 * DFF])
    cw_d = dt_in("conv_w", [L_TOTAL, 3, 2 * DFF])
    cb_d = dt_in("conv_b", [L_TOTAL, 2 * DFF])
    wdn_d = dt_in("w_down", [L_TOTAL, DFF, D])
    lnf_d = dt_in("lnf_g", [D])
    ident_d = dt_in("ident", [128, 128])
    cmask_d = dt_in("cmask", [128, 512])
    rmask_d = dt_in("rmask", [128, 512])
    y_d = nc.dram_tensor("y", [n_seq, seq_len, D], F32, kind="ExternalOutput").ap()

    es = ExitStack()
    sb = lambda name, shape, dt: es.enter_context(nc.sbuf_tensor(name, shape, dt))
    X = sb("X", [128, KC, T], F32)
    H = sb("H", [128, KC, T], BF16)
    POOL = sb("POOL", [128, NPAGES, 512], F32)
    POOLB = POOL[:].bitcast(BF16)
    S = sb("S", [128, L_TOTAL, 6, 128], F32)
    HALO = sb("HALO", [128, L_TOTAL, 44, 2], F32)
    RING = sb("RING", [128, NSLOT, KC, 512], BF16)
    DEC = sb("DEC", [128, 6, 8], F32)
    IDF = sb("IDF", [128, 128], F32)
    IDB = sb("IDB", [128, 128], BF16)
    ONESB = sb("ONESB", [128, 128], BF16)
    CMASK = sb("CMASK", [128, 512], F32)
    RMASK = sb("RMASK", [128, 512], F32)
    G1 = sb("G1", [128, L_TOTAL * 8], F32)
    G2 = sb("G2", [128, L_TOTAL * 8], F32)
    LNF = sb("LNF", [128, 8], F32)
    NBGK = sb("NBGK", [128, L_TOTAL * 2], F32)
    GNA = sb("GNA", [128, L_TOTAL], F32)
    GNB = sb("GNB", [128, L_TOTAL], F32)
    CW = sb("CW", [128, L_TOTAL * 3 * 44], F32)
    CB = sb("CB", [128, L_TOTAL * 44], F32)
    LBP = sb("LBP", [128, L_TOTAL * 4], F32)
    OML = sb("OML", [128, L_TOTAL * 4], F32)
    BADA = sb("BADA", [128, L_TOTAL * 48], F32)
    CT = sb("CT", [128, n_seq * 8], F32)
    MOD = sb("MOD", [128, L_TOTAL, 48, n_seq], F32)
    GS1 = sb("GS1", [128, L_TOTAL, 8, n_seq], F32)
    GS2 = sb("GS2", [128, L_TOTAL, 8, n_seq], F32)
    WGK = sb("WGK", [16, L_TOTAL, 256], F32)
    CONST = sb("CONST", [128, 8], F32)
    SM = sb("SM", [128, 64], F32)
    ROWS = sb("ROWS", [128, 128], F32)
    HM = sb("HM", [128, 2], F32)
    STMP = sb("STMP", [128, 6, 128], F32)
    CORR = sb("CORR", [128, 44, 2], F32)
    CORT = sb("CORT", [128, 44], F32)
    PS = es.enter_context(nc.psum_tensor("PS", [128, 8, 512], F32))
    PSB = PS[:].bitcast(BF16)

    P = Prog(nc)
    bank_ctr = [0]
    ROT = [list(range(8))]

    def nb():
        r = ROT[0]
        b = r[bank_ctr[0] % len(r)]
        bank_ctr[0] += 1
        return b

    def run_tasks(tasks):
        tasks = [iter(t) for t in tasks]
        while tasks:
            for t in list(tasks):
                try:
                    next(t)
                except StopIteration:
                    tasks.remove(t)

    pk = lambda i: ('pg', i)
    pks = lambda i0, n: [('pg', i) for i in range(i0, i0 + n)]

    def b16(p0, c, n=1):
        page, half = p0 + c // 2, c % 2
        return POOLB[:, page, half * 512:half * 512 + 512 * n]

    P_QA, P_QB, P_KB, P_KA, P_SB16 = 0, 2, 3, 4, 4
    P_GT, P_OG = 10, 10
    P_RSTD, P_LNV, P_TMP0, P_TMP1 = 10, 11, 12, 13
    P_LF, P_BC, P_D1, P_D4, P_E0, P_E1 = 10, 11, 12, 13, 14, 15
    P_QT, P_SQ, P_KT, P_QH, P_KHT, P_KHM = 16, 16, 20, 23, 27, 28
    P_VA, P_VB, P_SGA, P_SGB, P_SCT = 34, 36, 38, 40, 42
    P_TMPS, P_OSB, P_SQH, P_RB = 46, 47, 48, 49
    P_XT = 16
    P_G, P_CV = 16, 0

    eps_ap = CONST[:, 0:1]
    one_ap = CONST[:, 1:2]
    ln8_ap = CONST[:, 2:3]

    P.op('dve', lambda e: e.memset(CONST[:, 0:1], EPS), writes=['CONST'])
    P.op('dve', lambda e: e.memset(CONST[:, 1:2], 1.0), writes=['CONST'])
    P.op('dve', lambda e: e.memset(CONST[:, 2:3], math.log(0.125)), writes=['CONST'])
    P.op('dve', lambda e: e.memset(CONST[:, 3:4], 0.0), writes=['CONST'])
    P.op('dve', lambda e: e.memset(CONST[:, 4:5], -QSHIFT), writes=['CONST'])
    P.op('dve', lambda e: e.memset(CONST[:, 5:6], math.log(0.125) - QSHIFT), writes=['CONST'])
    P.op('dve', lambda e: e.memset(ONESB[:], 1.0), writes=['ONESB'])
    P.op('dve', lambda e: e.memset(HM[:], 0.0), writes=['HM'])
    P.op('dve', lambda e: e.memset(HM[0:64, 0:1], 1.0), writes=['HM'])
    P.op('dve', lambda e: e.memset(HM[64:128, 1:2], 1.0), writes=['HM'])
    P.op('dve', lambda e: e.memset(POOL[:, 0:25, :], 0.0), writes=pks(0, 25))
    P.op('dve', lambda e: e.memset(POOL[:, 25:NPAGES, :], 0.0), writes=pks(25, NPAGES - 25))
    P.op('sp', lambda e: e.dma_start(out=IDF[:], in_=ident_d), writes=['IDF'], dma='c0')
    P.op('sp', lambda e: e.dma_start(out=CMASK[:], in_=cmask_d), writes=['CMASK'], dma='c1')
    P.op('sp', lambda e: e.dma_start(out=RMASK[:], in_=rmask_d), writes=['RMASK'], dma='c2')
    P.op('sp', lambda e: e.dma_start(out=WGK[:], in_=wgk_d.rearrange("l r k -> r l k")), writes=['WGK'], dma='c3')
    P.op('act', lambda e: e.activation(out=IDB[:], in_=IDF[:], func=AF.Copy), reads=['IDF'], writes=['IDB'])

    def load_rows_T(dst, dst_key, src_rows, R):
        P.op('sp', lambda e: e.dma_start(out=ROWS[0:R, :], in_=src_rows), writes=['ROWS'], dma='rows')
        b = nb()
        P.op('pe', lambda e: e.transpose(out=PS[:, b, 0:R], in_=ROWS[0:R, :], identity=IDF[0:R, 0:R]),
             reads=['ROWS', 'IDF'], writes=[('ps', b)])
        P.op('act', lambda e: e.activation(out=dst, in_=PS[:, b, 0:R], func=AF.Copy),
             reads=[('ps', b)], writes=[dst_key])

    def load_param(dst_tile, key, src2d, total_rows):
        r0 = 0
        while r0 < total_rows:
            R = min(128, total_rows - r0)
            load_rows_T(dst_tile[:, r0:r0 + R], key, src2d[r0:r0 + R, :], R)
            r0 += R

    load_param(G1, 'G1', ln1_d.rearrange("l (c p) -> (l c) p", p=128), L_TOTAL * 8)
    load_param(G2, 'G2', ln2_d.rearrange("l (c p) -> (l c) p", p=128), L_TOTAL * 8)
    load_param(LNF, 'LNF', lnf_d.rearrange("(c p) -> c p", p=128), 8)
    load_param(NBGK, 'NBGK', bgk_d.rearrange("l (c p) -> (l c) p", p=128), L_TOTAL * 2)
    load_param(GNA, 'GNA', gna_d, L_TOTAL)
    load_param(GNB, 'GNB', gnb_d, L_TOTAL)
    load_param(CW, 'CW', cw_d.rearrange("l j (m p) -> (l j m) p", p=128), L_TOTAL * 3 * 44)
    load_param(CB, 'CB', cb_d.rearrange("l (m p) -> (l m) p", p=128), L_TOTAL * 44)
    load_param(LBP, 'LBP', lbp_d.rearrange("l (c p) -> (l c) p", p=128), L_TOTAL * 4)
    load_param(BADA, 'BADA', bada_d.rearrange("l (c p) -> (l c) p", p=128), L_TOTAL * 48)
    load_param(CT, 'CT', c_d.rearrange("s (c p) -> (s c) p", p=128), n_seq * 8)
    P.op('dve', lambda e: e.tensor_scalar(out=NBGK[:], in0=NBGK[:], scalar1=-1.0, scalar2=None, op0=ALU.mult),
         reads=['NBGK'], writes=['NBGK'])
    P.op('act', lambda e: e.activation(out=CT[:], in_=CT[:], func=AF.Silu), reads=['CT'], writes=['CT'])
    lb4 = LBP[:].rearrange("p (l c) -> p l c", c=4)
    mx, ex, sm_, rs_ = SM[:, 0:4], SM[:, 4:20].rearrange("p (l c) -> p l c", c=4), SM[:, 20:24], SM[:, 24:28]
    P.op('dve', lambda e: e.tensor_tensor(out=mx, in0=lb4[:, 0, :], in1=lb4[:, 1, :], op=ALU.max), reads=['LBP'], writes=['SM'])
    P.op('dve', lambda e: e.tensor_tensor(out=mx, in0=mx, in1=lb4[:, 2, :], op=ALU.max), reads=['LBP', 'SM'], writes=['SM'])
    P.op('dve', lambda e: e.tensor_tensor(out=mx, in0=mx, in1=lb4[:, 3, :], op=ALU.max), reads=['LBP', 'SM'], writes=['SM'])
    for l in range(4):
        P.op('dve', lambda e, l=l: e.tensor_tensor(out=ex[:, l, :], in0=lb4[:, l, :], in1=mx, op=ALU.subtract),
             reads=['LBP', 'SM'], writes=['SM'])
    P.op('act', lambda e: e.activation(out=SM[:, 4:20], in_=SM[:, 4:20], func=AF.Exp), reads=['SM'], writes=['SM'])
    P.op('dve', lambda e: e.tensor_tensor(out=sm_, in0=ex[:, 0, :], in1=ex[:, 1, :], op=ALU.add), reads=['SM'], writes=['SM'])
    P.op('dve', lambda e: e.tensor_tensor(out=sm_, in0=sm_, in1=ex[:, 2, :], op=ALU.add), reads=['SM'], writes=['SM'])
    P.op('dve', lambda e: e.tensor_tensor(out=sm_, in0=sm_, in1=ex[:, 3, :], op=ALU.add), reads=['SM'], writes=['SM'])
    P.op('dve', lambda e: e.reciprocal(out=rs_, in_=sm_), reads=['SM'], writes=['SM'])
    oml4 = OML[:].rearrange("p (l c) -> p l c", c=4)
    P.op('dve', lambda e: e.memset(OML[:], 1.0), writes=['OML'])
    for l in range(1, 4):
        P.op('dve', lambda e, l=l: e.tensor_tensor(out=ex[:, l, :], in0=ex[:, l, :], in1=rs_, op=ALU.mult),
             reads=['SM'], writes=['SM'])
        P.op('dve', lambda e, l=l: e.tensor_tensor(out=oml4[:, l, :], in0=oml4[:, l - 1, :], in1=ex[:, l, :], op=ALU.subtract),
             reads=['SM', 'OML'], writes=['OML'])

    ct3 = CT[:].rearrange("p (s c) -> p s c", c=8)
    wa_bufs = [(POOL[:, 0:12, :], pks(0, 12)), (POOL[:, 12:24, :], pks(12, 12))]
    wa_i = 0
    for l in layers:
        b = nb()
        for q in range(8):
            buf, bkeys = wa_bufs[wa_i % 2]
            wa_i += 1
            bufv = buf.rearrange("p a n -> p (a n)").rearrange("p (k n) -> p k n", k=8)
            src = wada_d[l].rearrange("(k p) n -> p k n", p=128)[:, :, q * 768:(q + 1) * 768]
            P.op('sp', lambda e, bufv=bufv, src=src: e.dma_start(out=bufv, in_=src), writes=bkeys, dma='wa%d' % (wa_i % 2))
            for j6 in range(6):
                jc = q * 6 + j6
                for kc in range(8):
                    P.op('pe', lambda e, bufv=bufv, j6=j6, jc=jc, kc=kc, b=b: e.matmul(
                        PS[:, b, jc * n_seq:(jc + 1) * n_seq], lhsT=bufv[:, kc, j6 * 128:(j6 + 1) * 128],
                        rhs=ct3[:, :, kc], start=(kc == 0), stop=(kc == 7)),
                        reads=bkeys + ['CT'], writes=[('ps', b)])
        bada3 = BADA[:, l * 48:(l + 1) * 48]
        P.op('dve', lambda e, l=l, b=b, bada3=bada3: e.tensor_tensor(
            out=MOD[:, l, :, :], in0=PS[:, b, 0:48 * n_seq].rearrange("p (j s) -> p j s", s=n_seq),
            in1=bada3.unsqueeze(2).to_broadcast([128, 48, n_seq]), op=ALU.add),
            reads=[('ps', b), 'BADA'], writes=['MOD'])
        for (GS, Gp, joff) in ((GS1, G1, 1), (GS2, G2, 4)):
            P.op('dve', lambda e, l=l, GS=GS, joff=joff: e.tensor_scalar(
                out=GS[:, l, :, :], in0=MOD[:, l, joff * 8:(joff + 1) * 8, :], scalar1=1.0, scalar2=None, op0=ALU.add),
                reads=['MOD'], writes=['GS'])
            P.op('dve', lambda e, l=l, GS=GS, Gp=Gp: e.tensor_tensor(
                out=GS[:, l, :, :], in0=GS[:, l, :, :],
                in1=Gp[:, l * 8:(l + 1) * 8].unsqueeze(2).to_broadcast([128, 8, n_seq]), op=ALU.mult),
                reads=['GS', 'G1', 'G2'], writes=['GS'])
    P.op('dve', lambda e: e.memset(POOL[:, 0:24, :], 0.0), writes=pks(0, 24))

    wloads = []

    def plan_weights():
        for s in range(n_seq):
            for t in range(n_tiles):
                for l in layers:
                    wv = win_d[l].rearrange("(k p) n -> p k n", p=128)
                    for ci in (1, 0, 7, 4, 2, 5, 3, 6):
                        if ci == 7:
                            wloads.append([(lambda sl: RING[:, sl, :, 0:16], wv[:, :, 3584:3600])])
                        else:
                            wloads.append([(lambda sl: RING[:, sl, :, :], wv[:, :, ci * 512:(ci + 1) * 512])])
                    ov = wout_d[l].rearrange("(k p) n -> p k n", p=128)
                    for ci in range(2):
                        wloads.append([(lambda sl: RING[:, sl, :, :], ov[:, :, ci * 512:(ci + 1) * 512])])
                    uv = wup_d[l].rearrange("(k p) n -> p k n", p=128)
                    for i in range(11):
                        wloads.append([
                            (lambda sl: RING[:, sl, :, 0:256], uv[:, :, 256 * i:256 * i + 256]),
                            (lambda sl: RING[:, sl, :, 256:512], uv[:, :, DFF + 256 * i:DFF + 256 * i + 256])])
                    dv = wdn_d[l].rearrange("(k p) n -> p k n", p=128)
                    for hf in range(2):
                        for g3 in range(3):
                            k0 = g3 * 8
                            k1 = min(22, k0 + 8)
                            wloads.append([(lambda sl, k0=k0, k1=k1: RING[:, sl, 0:k1 - k0, :],
                                            dv[:, k0:k1, hf * 512:(hf + 1) * 512])])
    plan_weights()
    wstate = {'issued': 0, 'consumed': 0}

    def issue_load():
        i = wstate['issued']
        if i >= len(wloads):
            return
        sl = i % NSLOT
        for (dstf, src) in wloads[i]:
            dst = dstf(sl)
            P.op('pool', lambda e, dst=dst, src=src: e.dma_start(out=dst, in_=src), writes=[('W', sl)], dma='w%d' % sl)
        wstate['issued'] += 1

    def next_w():
        i = wstate['consumed']
        wstate['consumed'] += 1
        return i % NSLOT

    def release_w():
        issue_load()

    for _ in range(NSLOT):
        issue_load()

    Hkeys = [('H', k) for k in range(8)]
    Xkeys = [('X', k) for k in range(8)]

    def norm_mod(src_keys_fn, l, s, GS, shoff):
        for dc in range(8):
            P.op('act', lambda e, dc=dc: e.activation(out=b16(P_SQ, dc), in_=X[:, dc, :], func=AF.Square),
                 reads=[('X', dc)], writes=[pk(P_SQ + dc // 2)])
        b = nb()
        for dc in range(8):
            P.op('pe', lambda e, dc=dc, b=b: e.matmul(PS[:, b, :], lhsT=ONESB[:], rhs=b16(P_SQ, dc),
                                                     start=(dc == 0), stop=(dc == 7)),
                 reads=[pk(P_SQ + dc // 2), 'ONESB'], writes=[('ps', b)])
        P.op('act', lambda e, b=b: e.activation(out=POOL[:, P_LNV, :], in_=PS[:, b, :], func=AF.Ln, scale=1.0 / D, bias=eps_ap),
             reads=[('ps', b), 'CONST'], writes=[pk(P_LNV)])
        P.op('act', lambda e: e.activation(out=POOL[:, P_RSTD, :], in_=POOL[:, P_LNV, :], func=AF.Exp, scale=-0.5),
             reads=[pk(P_LNV)], writes=[pk(P_RSTD)])
        for dc in range(8):
            tp = P_TMP0 + (dc % 2)
            P.op('dve', lambda e, dc=dc, tp=tp: e.tensor_tensor(out=POOL[:, tp, :], in0=X[:, dc, :], in1=POOL[:, P_RSTD, :], op=ALU.mult),
                 reads=[('X', dc), pk(P_RSTD)], writes=[pk(tp)])
            P.op('act', lambda e, dc=dc, tp=tp: e.activation(out=H[:, dc, :], in_=POOL[:, tp, :], func=AF.Identity,
                                                            scale=GS[:, l, dc, s:s + 1], bias=MOD[:, l, shoff * 8 + dc, s:s + 1]),
                 reads=[pk(tp), 'GS', 'MOD'], writes=[('H', dc)])

    def mm_fm(sl, col0, ncols_chunks, evac):
        for ci in range(ncols_chunks):
            b = nb()
            for kc in range(8):
                P.op('pe', lambda e, ci=ci, kc=kc, b=b: e.matmul(
                    PS[:, b, :], lhsT=RING[:, sl, kc, col0 + ci * 128:col0 + (ci + 1) * 128], rhs=H[:, kc, :],
                    start=(kc == 0), stop=(kc == 7)),
                    reads=[('W', sl), ('H', kc)], writes=[('ps', b)])
            evac(ci, b)
            yield

    def mm_tm(sl, dst_page):
        for blk in range(NBLK):
            b = nb()
            for kc in range(8):
                P.op('pe', lambda e, kc=kc, b=b, blk=blk: e.matmul(
                    PS[:, b, :], lhsT=H[:, kc, blk * 128:(blk + 1) * 128], rhs=RING[:, sl, kc, :],
                    start=(kc == 0), stop=(kc == 7)),
                    reads=[('W', sl), ('H', kc)], writes=[('ps', b)])
            P.op('act', lambda e, b=b, blk=blk: e.activation(out=b16(dst_page, blk), in_=PS[:, b, :], func=AF.Copy),
                 reads=[('ps', b)], writes=[pk(dst_page + blk // 2)])
            yield

    TSETS = [dict(LF=10, BC=11, D1=12, D4=13, E=[14, 15, 8, 9]), dict(LF=42, BC=43, D1=44, D4=45, E=[46, 47, 48, 50])]

    def gate_chunk(fc, l, ts, gflags):
        T_LF, T_BC, T_D1, T_D4 = ts['LF'], ts['BC'], ts['D1'], ts['D4']
        T_E = ts['E']
        if fc < 4:
            hc = fc
            sigma, qb_ap, qt_ap = 1.0, CONST[:, 3:4], CONST[:, 4:5]
            q_ap, k_ap = b16(P_QA, hc), POOL[:, P_KA + hc, :]
            qk_keys = [pk(P_QA + hc // 2), pk(P_KA + hc)]
            P.op('dve', lambda e: e.tensor_scalar(out=POOL[:, P_KA + hc, :], in0=POOL[:, P_KA + hc, :],
                                                  scalar1=OML[:, l * 4 + hc:l * 4 + hc + 1], scalar2=None, op0=ALU.mult),
                 reads=[pk(P_KA + hc), 'OML'], writes=[pk(P_KA + hc)])
            P.op('act', lambda e: e.activation(out=POOL[:, T_LF, :], in_=POOL[:, P_KA + hc, :], func=AF.Ln, scale=-1.0, bias=one_ap),
                 reads=[pk(P_KA + hc), 'CONST'], writes=[pk(T_LF)])
        else:
            c2 = fc - 4
            sigma, qb_ap, qt_ap = -1.0 / 16.0, ln8_ap, CONST[:, 5:6]
            q_ap, k_ap = b16(P_QB, c2), b16(P_KB, c2)
            qk_keys = [pk(P_QB), pk(P_KB)]
            bg = nb()
            P.op('pe', lambda e: e.matmul(PS[:, bg, :], lhsT=WGK[0:16, l, c2 * 128:(c2 + 1) * 128], rhs=POOL[0:16, P_RB, :],
                                          start=True, stop=True),
                 reads=[pk(P_RB), 'WGK'], writes=[('ps', bg)])
            P.op('act', lambda e: e.activation(out=POOL[:, T_E[0], :], in_=PS[:, bg, :], func=AF.Exp, scale=-1.0,
                                               bias=NBGK[:, l * 2 + c2:l * 2 + c2 + 1]),
                 reads=[('ps', bg), 'NBGK'], writes=[pk(T_E[0])])
            P.op('act', lambda e: e.activation(out=POOL[:, T_LF, :], in_=POOL[:, T_E[0], :], func=AF.Ln, bias=one_ap),
                 reads=[pk(T_E[0]), 'CONST'], writes=[pk(T_LF)])
        yield
        bc3 = POOL[:, T_BC, :].rearrange("p (c t) -> p c t", t=64)
        P.op('dve', lambda e: e.tensor_tensor_scan(out=POOL[:, T_BC, :], data0=RMASK[:], data1=POOL[:, T_LF, :],
                                                   initial=0.0, op0=ALU.mult, op1=ALU.add),
             reads=[pk(T_LF), 'RMASK'], writes=[pk(T_BC)])
        yield
        P.op('dve', lambda e: e.tensor_tensor(out=POOL[:, T_D1, :].rearrange("p (c t) -> p c t", t=64), in0=bc3,
                                              in1=bc3[:, :, 32:33].to_broadcast([128, 8, 64]), op=ALU.subtract),
             reads=[pk(T_BC)], writes=[pk(T_D1)])
        P.op('act', lambda e: e.activation(out=DEC[:, fc, :], in_=POOL[:, T_BC, 63:512:64], func=AF.Exp, scale=sigma),
             reads=[pk(T_BC)], writes=[('DEC', fc)])
        yield
        P.op('dve', lambda e: e.tensor_tensor(out=POOL[:, T_D4, :].rearrange("p (c t) -> p c t", t=64),
                                              in0=bc3[:, :, 63:64].to_broadcast([128, 8, 64]), in1=bc3, op=ALU.subtract),
             reads=[pk(T_BC)], writes=[pk(T_D4)])
        yield
        plan = [
            (T_D4, sigma, CONST[:, 3:4], 'k', 'KHT'),
            (T_D1, -sigma, CONST[:, 3:4], 'k', 'KT'),
            (T_D1, sigma, qt_ap, 'q', 'QT'),
            (T_BC, sigma, qb_ap, 'q', 'QH'),
        ]
        for i, (src, sc, bias, which, kind) in enumerate(plan):
            if which == 'q' and fc < 4:
                while not gflags.get('qa'):
                    yield
            ep = T_E[i]
            P.op('act', lambda e, src=src, sc=sc, bias=bias, ep=ep: e.activation(
                out=POOL[:, ep, :], in_=POOL[:, src, :], func=AF.Exp, scale=sc, bias=bias),
                reads=[pk(src), 'CONST'], writes=[pk(ep)])
            yield
            src_ap = q_ap if which == 'q' else k_ap
            if kind in ('QT', 'QH'):
                p0 = P_QT if kind == 'QT' else P_QH
                if fc < 4:
                    P.op('dve', lambda e, ep=ep, src_ap=src_ap, p0=p0: e.tensor_tensor(
                        out=b16(p0, fc), in0=src_ap, in1=POOL[:, ep, :], op=ALU.mult),
                        reads=[pk(ep)] + qk_keys, writes=[pk(p0 + fc // 2)])
                else:
                    for hh in range(2):
                        g = (fc - 4) * 2 + hh
                        P.op('dve', lambda e, ep=ep, src_ap=src_ap, p0=p0, g=g, hh=hh: e.scalar_tensor_tensor(
                            out=b16(p0, 4 + g), in0=src_ap, scalar=HM[:, hh:hh + 1], in1=POOL[:, ep, :],
                            op0=ALU.mult, op1=ALU.mult),
                            reads=[pk(ep), 'HM'] + qk_keys, writes=[pk(p0 + (4 + g) // 2)])
            elif kind == 'KT':
                P.op('dve', lambda e, ep=ep, src_ap=src_ap: e.tensor_tensor(
                    out=b16(P_KT, fc), in0=src_ap, in1=POOL[:, ep, :], op=ALU.mult),
                    reads=[pk(ep)] + qk_keys, writes=[pk(P_KT + fc // 2)])
            else:
                kht = b16(P_KHT, fc % 2)
                P.op('dve', lambda e, ep=ep, src_ap=src_ap, kht=kht: e.tensor_tensor(
                    out=kht, in0=src_ap, in1=POOL[:, ep, :], op=ALU.mult),
                    reads=[pk(ep)] + qk_keys, writes=[pk(P_KHT)])
                yield
                b = nb()
                for blk in range(NBLK):
                    P.op('pe', lambda e, b=b, blk=blk, kht=kht: e.transpose(
                        out=PSB[:, b, blk * 128:(blk + 1) * 128], in_=kht[:, blk * 128:(blk + 1) * 128], identity=IDB[:]),
                        reads=[pk(P_KHT), 'IDB'], writes=[('ps', b)])
                khm = POOLB[:, P_KHM + fc, :].rearrange("p (j d) -> p j d", d=128)
                pv = PSB[:, b, 0:512].rearrange("p (k d) -> p k d", d=128)
                P.op('act', lambda e, khm=khm, pv=pv, b=b: e.activation(out=khm[0:64, 0:8:2, :], in_=pv[0:64, :, :], func=AF.Copy),
                     reads=[('ps', b)], writes=[pk(P_KHM + fc)])
                P.op('act', lambda e, khm=khm, pv=pv, b=b: e.activation(out=khm[64:128, 1:8:2, :], in_=pv[64:128, :, :], func=AF.Copy),
                     reads=[('ps', b)], writes=[pk(P_KHM + fc)])
            yield

    def layer_step(l, s, t, first_tile):
        norm_mod(None, l, s, GS1, 0)
        flags = {}

        def ev_qa(ci, b):
            P.op('act', lambda e: e.activation(out=b16(P_QA, ci), in_=PS[:, b, :], func=AF.Copy),
                 reads=[('ps', b)], writes=[pk(P_QA + ci // 2)])

        def ev_fa(ci, b):
            P.op('act', lambda e: e.activation(out=POOL[:, P_KA + ci, :], in_=PS[:, b, :], func=AF.Sigmoid, scale=-1.0),
                 reads=[('ps', b)], writes=[pk(P_KA + ci)])

        def ev_sga(ci, b):
            P.op('act', lambda e: e.activation(out=b16(P_SGA, ci), in_=PS[:, b, :], func=AF.Copy),
                 reads=[('ps', b)], writes=[pk(P_SGA + ci // 2)])

        def ev_sgb(ci, b):
            P.op('act', lambda e: e.activation(out=b16(P_SGB, ci), in_=PS[:, b, :], func=AF.Copy),
                 reads=[('ps', b)], writes=[pk(P_SGB + ci // 2)])

        def ev_qk(ci, b):
            if ci < 2:
                P.op('act', lambda e: e.activation(out=b16(P_QB, ci), in_=PS[:, b, :], func=AF.Copy),
                     reads=[('ps', b)], writes=[pk(P_QB)])
            else:
                P.op('act', lambda e: e.activation(out=b16(P_KB, ci - 2), in_=PS[:, b, :], func=AF.Copy),
                     reads=[('ps', b)], writes=[pk(P_KB)])

        sl = next_w()
        for _ in mm_fm(sl, 0, 4, ev_fa):
            pass
        release_w()
        def main_task():
            sl = next_w()
            yield from mm_fm(sl, 0, 4, ev_qa)
            release_w()
            flags['qa'] = True
            sl = next_w()
            b = nb()
            for kc in range(8):
                P.op('pe', lambda e, kc=kc, b=b, sl=sl: e.matmul(PS[0:16, b, :], lhsT=RING[:, sl, kc, 0:16], rhs=H[:, kc, :],
                                                                start=(kc == 0), stop=(kc == 7)),
                     reads=[('W', sl), ('H', kc)], writes=[('ps', b)])
            P.op('act', lambda e, b=b: e.activation(out=POOL[0:16, P_RB, :], in_=PS[0:16, b, :], func=AF.Copy),
                 reads=[('ps', b)], writes=[pk(P_RB)])
            release_w()
            yield
            sl = next_w()
            yield from mm_fm(sl, 0, 4, ev_qk)
            release_w()
            flags['gla'] = True
            sl = next_w()
            yield from mm_tm(sl, P_VA)
            release_w()
            sl = next_w()
            yield from mm_tm(sl, P_VB)
            release_w()
            sl = next_w()
            yield from mm_fm(sl, 0, 4, ev_sga)
            release_w()
            sl = next_w()
            yield from mm_fm(sl, 0, 4, ev_sgb)
            release_w()

        def gate_task(fcs, ts):
            for fc in fcs:
                if fc >= 4:
                    while not flags.get('gla'):
                        yield
                yield from gate_chunk(fc, l, ts, flags)

        run_tasks([main_task(), gate_task([0, 2, 4], TSETS[0]), gate_task([1, 3, 5], TSETS[1])])

        def head_info(h):
            if h < 4:
                return dict(fc=h, qt=b16(P_QT, h), qtk=pk(P_QT + h // 2), kt=b16(P_KT, h), ktk=pk(P_KT + h // 2),
                            qh=b16(P_QH, h), qhk=pk(P_QH + h // 2), vpage=P_VA, vcol=h * 128,
                            gn=GNA[:, l:l + 1], sg=b16(P_SGA, h), sgk=pk(P_SGA + h // 2))
            g = h - 4
            fc = 4 + g // 2
            return dict(fc=fc, qt=b16(P_QT, 4 + g), qtk=pk(P_QT + (4 + g) // 2), kt=b16(P_KT, fc), ktk=pk(P_KT + fc // 2),
                        qh=b16(P_QH, 4 + g), qhk=pk(P_QH + (4 + g) // 2), vpage=P_VB, vcol=g * 128,
                        gn=GNB[:, l:l + 1], sg=b16(P_SGB, g), sgk=pk(P_SGB + g // 2))

        def scores_task():
            for h in range(8):
                hi = head_info(h)
                b = nb()
                for blk in range(NBLK):
                    P.op('pe', lambda e, hi=hi, b=b, blk=blk: e.matmul(
                        PS[:, b, blk * 128:(blk + 1) * 128], lhsT=hi['kt'][:, blk * 128:(blk + 1) * 128],
                        rhs=hi['qt'][:, blk * 128:(blk + 1) * 128], start=True, stop=True),
                        reads=[hi['ktk'], hi['qtk']], writes=[('ps', b)])
                P.op('dve', lambda e, b=b: e.tensor_scalar(out=POOL[:, P_TMPS, :], in0=PS[:, b, :], scalar1=1e30, scalar2=-1e30,
                                                           op0=ALU.min, op1=ALU.max),
                     reads=[('ps', b)], writes=[pk(P_TMPS)])
                P.op('pool', lambda e, h=h: e.tensor_tensor(out=b16(P_SCT, h), in0=POOL[:, P_TMPS, :], in1=CMASK[:], op=ALU.mult),
                     reads=[pk(P_TMPS), 'CMASK'], writes=[pk(P_SCT + h // 2)])
                yield

        def state_task(fcs, banks):
            for i, fc in enumerate(fcs):
                skey = ('S', l, fc)
                if first_tile:
                    P.op('dve', lambda e, fc=fc: e.memset(S[:, l, fc, :], 0.0), writes=[skey])
                for j in range(NCH):
                    bnk = banks[2 * i + (j % 2)]
                    reg = (j // 2) * 128
                    if fc < 4:
                        P.op('pe', lambda e, fc=fc, j=j, bnk=bnk, reg=reg: e.matmul(
                            PS[:, bnk, reg:reg + 128], lhsT=POOLB[:, P_KHM + fc, j * 128:(j + 1) * 128],
                            rhs=b16(P_VA, j // 2)[:, fc * 128:(fc + 1) * 128], start=True, stop=True),
                            reads=[pk(P_KHM + fc), pk(P_VA + (j // 2) // 2)], writes=[('ps', bnk)])
                    else:
                        for hh in range(2):
                            g = (fc - 4) * 2 + hh
                            P.op('pe', lambda e, fc=fc, j=j, bnk=bnk, reg=reg, hh=hh, g=g: e.matmul(
                                PS[hh * 64:(hh + 1) * 64, bnk, reg:reg + 128],
                                lhsT=POOLB[:, P_KHM + fc, j * 128 + hh * 64:j * 128 + (hh + 1) * 64],
                                rhs=b16(P_VB, j // 2)[:, g * 128:(g + 1) * 128], start=True, stop=True),
                                reads=[pk(P_KHM + fc), pk(P_VB + (j // 2) // 2)], writes=[('ps', bnk)])
                yield
            for j in range(NCH):
                for i, fc in enumerate(fcs):
                    sb16 = POOLB[:, P_SB16 + fc, :].rearrange("p (j e) -> p j e", e=128)
                    bnk = banks[2 * i + (j % 2)]
                    reg = (j // 2) * 128
                    if j % 2 == 0:
                        src, skey, dst, dkey = S[:, l, fc, :], ('S', l, fc), STMP[:, fc, :], ('STMP', fc)
                    else:
                        src, skey, dst, dkey = STMP[:, fc, :], ('STMP', fc), S[:, l, fc, :], ('S', l, fc)
                    P.op('act', lambda e, j=j, sb16=sb16, src=src: e.activation(out=sb16[:, j, :], in_=src, func=AF.Copy),
                         reads=[skey], writes=[pk(P_SB16 + fc)])
                    P.op('dve', lambda e, fc=fc, j=j, bnk=bnk, reg=reg, src=src, dst=dst: e.scalar_tensor_tensor(
                        out=dst, in0=src, scalar=DEC[:, fc, j:j + 1], in1=PS[:, bnk, reg:reg + 128],
                        op0=ALU.mult, op1=ALU.add),
                        reads=[skey, ('DEC', fc), ('ps', bnk)], writes=[dkey])
                yield

        def o_task(heads, tmps=P_TMPS, osb=P_OSB, sqh=0):
            for h in heads:
                hi = head_info(h)
                fc = hi['fc']
                sb16 = POOLB[:, P_SB16 + fc, :].rearrange("p (j e) -> p j e", e=128)
                b = nb()
                for blk in range(NBLK):
                    P.op('pe', lambda e, hi=hi, b=b, blk=blk, h=h: e.matmul(
                        PS[:, b, blk * 128:(blk + 1) * 128], lhsT=b16(hi['vpage'], blk)[:, hi['vcol']:hi['vcol'] + 128],
                        rhs=b16(P_SCT, h)[:, blk * 128:(blk + 1) * 128], start=True, stop=False),
                        reads=[pk(hi['vpage'] + blk // 2), pk(P_SCT + h // 2)], writes=[('ps', b)])
                    for jj in range(2):
                        j = blk * 2 + jj
                        P.op('pe', lambda e, hi=hi, b=b, j=j, sb16=sb16, jj=jj: e.matmul(
                            PS[:, b, j * 64:(j + 1) * 64], lhsT=sb16[:, j, :], rhs=hi['qh'][:, j * 64:(j + 1) * 64],
                            start=False, stop=(jj == 1)),
                            reads=[pk(P_SB16 + fc), hi['qhk']], writes=[('ps', b)])
                yield
                P.op('act', lambda e, b=b: e.activation(out=b16(P_SQH, sqh), in_=PS[:, b, :], func=AF.Square),
                     reads=[('ps', b)], writes=[pk(P_SQH)])
                b2 = nb()
                P.op('pe', lambda e, b2=b2: e.matmul(PS[:, b2, :], lhsT=ONESB[:], rhs=b16(P_SQH, sqh), start=True, stop=True),
                     reads=[pk(P_SQH), 'ONESB'], writes=[('ps', b2)])
                yield
                P.op('act', lambda e, b2=b2: e.activation(out=POOL[:, tmps, :], in_=PS[:, b2, :], func=AF.Ln, scale=1.0 / 128.0, bias=eps_ap),
                     reads=[('ps', b2), 'CONST'], writes=[pk(tmps)])
                yield
                P.op('act', lambda e: e.activation(out=POOL[:, tmps, :], in_=POOL[:, tmps, :], func=AF.Exp, scale=-0.5),
                     reads=[pk(tmps)], writes=[pk(tmps)])
                yield
                P.op('dve', lambda e, hi=hi, b=b: e.scalar_tensor_tensor(out=POOL[:, osb, :], in0=PS[:, b, :], scalar=hi['gn'],
                                                                        in1=POOL[:, tmps, :], op0=ALU.mult, op1=ALU.mult),
                     reads=[('ps', b), pk(tmps), 'GNA', 'GNB'], writes=[pk(osb)])
                yield
                P.op('pool', lambda e, hi=hi, h=h: e.tensor_tensor(out=b16(P_OG, h), in0=POOL[:, osb, :], in1=hi['sg'], op=ALU.mult),
                     reads=[pk(osb), hi['sgk']], writes=[pk(P_OG + h // 2)])
                yield

        def silu_task():
            for pg in (P_SGA, P_SGB):
                for ci in range(4):
                    P.op('act', lambda e, pg=pg, ci=ci: e.activation(out=b16(pg, ci), in_=b16(pg, ci), func=AF.Silu),
                         reads=[pk(pg + ci // 2)], writes=[pk(pg + ci // 2)])
                    yield

        ROT[0] = [6, 7]
        run_tasks([scores_task(), state_task([0, 1, 2], [0, 1, 2, 3, 4, 5]), silu_task()])
        run_tasks([state_task([3, 4, 5], [0, 1, 2, 3, 4, 5]), o_task([0, 1, 2])])
        ROT[0] = list(range(8))
        run_tasks([o_task([3, 5, 7]), o_task([4, 6], tmps=14, osb=15, sqh=1)])

        slots = [next_w(), next_w()]
        for n in range(8):
            sl = slots[n // 4]
            b = nb()
            for kc in range(8):
                P.op('pe', lambda e, n=n, kc=kc, b=b, sl=sl: e.matmul(
                    PS[:, b, :], lhsT=RING[:, sl, kc, (n % 4) * 128:(n % 4 + 1) * 128], rhs=b16(P_OG, kc),
                    start=(kc == 0), stop=(kc == 7)),
                    reads=[('W', sl), pk(P_OG + kc // 2)], writes=[('ps', b)])
            P.op('dve', lambda e, n=n, b=b: e.scalar_tensor_tensor(
                out=X[:, n, :], in0=PS[:, b, :], scalar=MOD[:, l, 2 * 8 + n, s:s + 1], in1=X[:, n, :], op0=ALU.mult, op1=ALU.add),
                reads=[('ps', b), ('X', n), 'MOD'], writes=[('X', n)])
            if n == 3:
                release_w()
        release_w()

        norm_mod(None, l, s, GS2, 3)

        hkey = ('HALO', l)
        if first_tile:
            P.op('dve', lambda e: e.memset(HALO[:, l, :, :], 0.0), writes=[hkey])
        W0 = CW[:, (l * 3 + 0) * 44:(l * 3 + 0) * 44 + 44]
        W1 = CW[:, (l * 3 + 1) * 44:(l * 3 + 1) * 44 + 44]
        P.op('dve', lambda e: e.tensor_tensor(out=CORR[:, :, 1], in0=W0, in1=HALO[:, l, :, 1], op=ALU.mult),
             reads=[hkey, 'CW'], writes=['CORR'])
        P.op('dve', lambda e: e.tensor_tensor(out=CORR[:, :, 0], in0=W0, in1=HALO[:, l, :, 0], op=ALU.mult),
             reads=[hkey, 'CW'], writes=['CORR'])
        P.op('dve', lambda e: e.tensor_tensor(out=CORT[:], in0=W1, in1=HALO[:, l, :, 1], op=ALU.mult),
             reads=[hkey, 'CW'], writes=['CORT'])
        P.op('dve', lambda e: e.tensor_tensor(out=CORR[:, :, 0], in0=CORR[:, :, 0], in1=CORT[:], op=ALU.add),
             reads=['CORR', 'CORT'], writes=['CORR'])

        def conv(b, m, ypage):
            w0 = CW[:, (l * 3 + 0) * 44 + m:(l * 3 + 0) * 44 + m + 1]
            w1 = CW[:, (l * 3 + 1) * 44 + m:(l * 3 + 1) * 44 + m + 1]
            w2 = CW[:, (l * 3 + 2) * 44 + m:(l * 3 + 2) * 44 + m + 1]
            cb = CB[:, l * 44 + m:l * 44 + m + 1]
            Y = POOL[:, ypage, :]
            P.op('act', lambda e: e.activation(out=Y, in_=PS[:, b, :], func=AF.Identity, scale=w2, bias=cb),
                 reads=[('ps', b), 'CW', 'CB'], writes=[pk(ypage)])
            P.op('dve', lambda e: e.scalar_tensor_tensor(out=Y[:, 1:512], in0=PS[:, b, 0:511], scalar=w1, in1=Y[:, 1:512],
                                                         op0=ALU.mult, op1=ALU.add),
                 reads=[('ps', b), pk(ypage), 'CW'], writes=[pk(ypage)])
            P.op('dve', lambda e: e.scalar_tensor_tensor(out=Y[:, 2:512], in0=PS[:, b, 0:510], scalar=w0, in1=Y[:, 2:512],
                                                         op0=ALU.mult, op1=ALU.add),
                 reads=[('ps', b), pk(ypage), 'CW'], writes=[pk(ypage)])
            P.op('dve', lambda e: e.tensor_tensor(out=Y[:, 0:2], in0=Y[:, 0:2], in1=CORR[:, m, :], op=ALU.add),
                 reads=['CORR', pk(ypage)], writes=[pk(ypage)])
            P.op('act', lambda e: e.activation(out=HALO[:, l, m, :], in_=PS[:, b, 510:512], func=AF.Copy),
                 reads=[('ps', b)], writes=[hkey])

        cvi = 0
        for i in range(11):
            sl = next_w()
            for q in range(2):
                m = 2 * i + q
                ya, yv, sa = P_CV + 3 * (cvi % 2), P_CV + 3 * (cvi % 2) + 1, P_CV + 3 * (cvi % 2) + 2
                cvi += 1
                ba = nb()
                for kc in range(8):
                    P.op('pe', lambda e, kc=kc, ba=ba, q=q, sl=sl: e.matmul(
                        PS[:, ba, :], lhsT=RING[:, sl, kc, q * 128:(q + 1) * 128], rhs=H[:, kc, :],
                        start=(kc == 0), stop=(kc == 7)),
                        reads=[('W', sl), ('H', kc)], writes=[('ps', ba)])
                bv = nb()
                for kc in range(8):
                    P.op('pe', lambda e, kc=kc, bv=bv, q=q, sl=sl: e.matmul(
                        PS[:, bv, :], lhsT=RING[:, sl, kc, 256 + q * 128:256 + (q + 1) * 128], rhs=H[:, kc, :],
                        start=(kc == 0), stop=(kc == 7)),
                        reads=[('W', sl), ('H', kc)], writes=[('ps', bv)])
                conv(ba, m, ya)
                conv(bv, NFF + m, yv)
                P.op('act', lambda e, ya=ya, sa=sa: e.activation(out=POOL[:, sa, :], in_=POOL[:, ya, :], func=AF.Silu),
                     reads=[pk(ya)], writes=[pk(sa)])
                P.op('pool', lambda e, sa=sa, yv=yv, m=m: e.tensor_tensor(out=b16(P_G, m), in0=POOL[:, sa, :], in1=POOL[:, yv, :], op=ALU.mult),
                     reads=[pk(sa), pk(yv)], writes=[pk(P_G + m // 2)])
            release_w()

        for hf in range(2):
            slots = [next_w(), next_w(), next_w()]
            for n4 in range(4):
                n = hf * 4 + n4
                b = nb()
                for kf in range(NFF):
                    sl = slots[kf // 8]
                    P.op('pe', lambda e, kf=kf, b=b, sl=sl, n4=n4: e.matmul(
                        PS[:, b, :], lhsT=RING[:, sl, kf % 8, n4 * 128:(n4 + 1) * 128], rhs=b16(P_G, kf),
                        start=(kf == 0), stop=(kf == NFF - 1)),
                        reads=[('W', sl), pk(P_G + kf // 2)], writes=[('ps', b)])
                P.op('dve', lambda e, n=n, b=b: e.scalar_tensor_tensor(
                    out=X[:, n, :], in0=PS[:, b, :], scalar=MOD[:, l, 5 * 8 + n, s:s + 1], in1=X[:, n, :], op0=ALU.mult, op1=ALU.add),
                    reads=[('ps', b), ('X', n), 'MOD'], writes=[('X', n)])
            release_w()
            release_w()
            release_w()

    XT = POOL[:, P_XT:P_XT + 8, :].rearrange("p (b a) n -> p b (a n)", a=2)
    for s in range(n_seq):
        for t in range(n_tiles):
            t0 = t * T
            for blk in range(NBLK):
                P.op('sp', lambda e, blk=blk, s=s, t0=t0: e.dma_start(out=XT[:, blk, :], in_=x_d[s, t0 + blk * 128:t0 + (blk + 1) * 128, :]),
                     writes=pks(P_XT + 2 * blk, 2), dma='xin%d' % blk)
            for dc in range(8):
                b = nb()
                for blk in range(NBLK):
                    P.op('pe', lambda e, b=b, blk=blk, dc=dc: e.transpose(
                        out=PS[:, b, blk * 128:(blk + 1) * 128], in_=XT[:, blk, dc * 128:(dc + 1) * 128], identity=IDF[:]),
                        reads=pks(P_XT + 2 * blk, 2) + ['IDF'], writes=[('ps', b)])
                P.op('act', lambda e, b=b, dc=dc: e.activation(out=X[:, dc, :], in_=PS[:, b, :], func=AF.Copy),
                     reads=[('ps', b)], writes=[('X', dc)])
            for l in layers:
                layer_step(l, s, t, t == 0)
            src_is_X = True
            if final_norm:
                for dc in range(8):
                    P.op('act', lambda e, dc=dc: e.activation(out=b16(P_SQ, dc), in_=X[:, dc, :], func=AF.Square),
                         reads=[('X', dc)], writes=[pk(P_SQ + dc // 2)])
                b = nb()
                for dc in range(8):
                    P.op('pe', lambda e, dc=dc, b=b: e.matmul(PS[:, b, :], lhsT=ONESB[:], rhs=b16(P_SQ, dc),
                                                             start=(dc == 0), stop=(dc == 7)),
                         reads=[pk(P_SQ + dc // 2), 'ONESB'], writes=[('ps', b)])
                P.op('act', lambda e, b=b: e.activation(out=POOL[:, P_LNV, :], in_=PS[:, b, :], func=AF.Ln, scale=1.0 / D, bias=eps_ap),
                     reads=[('ps', b), 'CONST'], writes=[pk(P_LNV)])
                P.op('act', lambda e: e.activation(out=POOL[:, P_RSTD, :], in_=POOL[:, P_LNV, :], func=AF.Exp, scale=-0.5),
                     reads=[pk(P_LNV)], writes=[pk(P_RSTD)])
                for dc in range(8):
                    P.op('dve', lambda e, dc=dc: e.scalar_tensor_tensor(
                        out=X[:, dc, :], in0=X[:, dc, :], scalar=LNF[:, dc:dc + 1], in1=POOL[:, P_RSTD, :], op0=ALU.mult, op1=ALU.mult),
                        reads=[('X', dc), pk(P_RSTD), 'LNF'], writes=[('X', dc)])
            for blk in range(NBLK):
                for half in range(2):
                    b = nb()
                    for d4 in range(4):
                        dc = half * 4 + d4
                        P.op('pe', lambda e, b=b, blk=blk, dc=dc, d4=d4: e.transpose(
                            out=PS[:, b, d4 * 128:(d4 + 1) * 128], in_=X[:, dc, blk * 128:(blk + 1) * 128], identity=IDF[:]),
                            reads=[('X', dc), 'IDF'], writes=[('ps', b)])
                    P.op('act', lambda e, b=b, blk=blk, half=half: e.activation(
                        out=XT[:, blk, half * 512:(half + 1) * 512], in_=PS[:, b, :], func=AF.Copy),
                        reads=[('ps', b)], writes=[pk(P_XT + 2 * blk + half)])
                P.op('sp', lambda e, blk=blk, s=s, t0=t0: e.dma_start(out=y_d[s, t0 + blk * 128:t0 + (blk + 1) * 128, :], in_=XT[:, blk, :]),
                     reads=pks(P_XT + 2 * blk, 2), writes=[('yout', blk)], dma='yout%d' % blk)
    P.wait_all('sp', [('yout', blk) for blk in range(NBLK)])
    P.emit()
    es.close()
    return nc


def _consts():
    ident = np.eye(128, dtype=np.float32)
    p = np.arange(128)[:, None]
    col = np.arange(512)[None, :]
    c = col % 128
    cmask = ((p // 64 == c // 64) & (p % 64 <= c % 64)).astype(np.float32) * np.float32(math.exp(QSHIFT))
    rmask = np.broadcast_to((col % 64 != 0).astype(np.float32), (128, 512)).copy()
    return ident, cmask, rmask


WEIGHT_NAMES = ["ln1_g", "ln2_g", "w_ada", "b_ada", "w_in", "lb_params", "w_gk", "b_gk", "gn_a", "gn_b",
                "w_out", "w_up", "conv_w", "conv_b", "w_down", "lnf_g"]


def run_launch(x, c, weights, layers, final_norm, n_cores):
    B, S_, _ = x.shape
    n_seq = B // n_cores
    nc = build_program(n_seq, S_, layers, True, final_norm)
    ident, cmask, rmask = _consts()
    in_maps = []
    for i in range(n_cores):
        m = {"x": np.ascontiguousarray(x[i * n_seq:(i + 1) * n_seq]),
             "c": np.ascontiguousarray(c[i * n_seq:(i + 1) * n_seq]),
             "ident": ident, "cmask": cmask, "rmask": rmask}
        for k in WEIGHT_NAMES:
            m[k] = weights[k]
        in_maps.append(m)
    res = run_bass_kernel_spmd(nc, in_maps, core_ids=list(range(n_cores)))
    return np.concatenate([r["y"] for r in res.results], axis=0)


FUSED = True


def kernel(**inputs):
    x = np.ascontiguousarray(np.asarray(inputs["x"], dtype=np.float32))
    c = np.ascontiguousarray(np.asarray(inputs["c"], dtype=np.float32))
    weights = {k: np.ascontiguousarray(np.asarray(inputs[k], dtype=np.float32)) for k in WEIGHT_NAMES}
    if FUSED:
        return run_launch(x, c, weights, [0, 1, 2, 3], True, 8)
    for l in range(4):
        x = run_launch(x, c, weights, [l], l == 3, 8)
    return x
```

```python
import math
from contextlib import ExitStack

import numpy as np
import concourse.bass as bass
import concourse.mybir as mybir
from concourse.bass_utils import run_bass_kernel_spmd

F32 = mybir.dt.float32
BF16 = mybir.dt.bfloat16
AF = mybir.ActivationFunctionType
ALU = mybir.AluOpType

D = 1024
KC = 8
T = 512
NBLK = 4
NCH = 8
L_TOTAL = 4
IN_DIM = 3600
DFF = 2816
NFF = 22
EPS = 1e-6
NSLOT = 6
NPAGES = 51
QSHIFT = 20.0


class Prog:
    CE = ('pe', 'act', 'dve', 'pool')

    def __init__(self, nc):
        self.nc = nc
        self.streams = {e: [] for e in ('pe', 'act', 'dve', 'pool', 'sp')}
        self.cnt = {}
        self.known = {e: {} for e in self.streams}
        self.bufs = {}
        self.semkeys = []

    def _sem(self, key):
        if key not in self.cnt:
            self.cnt[key] = 0
            self.semkeys.append(key)
        return key

    def _need(self, eng, clock, waits):
        if clock is None:
            return
        sk, val = clock
        if sk == eng and eng == 'pe':
            return
        if self.known[eng].get(sk, 0) >= val:
            return
        waits[sk] = max(waits.get(sk, 0), val)

    def op(self, eng, fn, reads=(), writes=(), dma=None):
        waits = {}
        for k in reads:
            b = self.bufs.get(k)
            if b:
                self._need(eng, b[0], waits)
        for k in writes:
            b = self.bufs.get(k)
            if b:
                self._need(eng, b[0], waits)
                for sk, v in b[1].items():
                    self._need(eng, (sk, v), waits)
        for sk, v in waits.items():
            self.known[eng][sk] = v
        if dma is not None:
            sk = self._sem(('dma', dma))
            self.cnt[sk] += 16
        else:
            sk = self._sem(eng)
            self.cnt[sk] += 1
        clock = (sk, self.cnt[sk])
        for k in reads:
            b = self.bufs.setdefault(k, [None, {}])
            b[1][sk] = max(b[1].get(sk, 0), clock[1])
        for k in writes:
            self.bufs[k] = [clock, {}]
        self.streams[eng].append((list(waits.items()), fn, sk, 16 if dma is not None else 1))
        return clock

    def wait_all(self, eng, keys):
        waits = {}
        for k in keys:
            b = self.bufs.get(k)
            if b:
                self._need(eng, b[0], waits)
        self.streams[eng].append((list(waits.items()), None, None, 0))

    def emit(self):
        nc = self.nc
        with ExitStack() as es:
            sems = {}
            for i, sk in enumerate(self.semkeys):
                sems[sk] = es.enter_context(nc.semaphore("s%d" % i))
            block = es.enter_context(nc.Block())

            def run(stream_name):
                def body(engine):
                    for waits, fn, sk, inc in self.streams[stream_name]:
                        for wk, wv in waits:
                            engine.wait_ge(sems[wk], wv)
                        if fn is not None:
                            fn(engine).then_inc(sems[sk], inc)
                return body
            block.tensor(run('pe'))
            block.scalar(run('act'))
            block.vector(run('dve'))
            block.gpsimd(run('pool'))
            block.sync(run('sp'))


def build_program(n_seq, seq_len, layers, first, final_norm):
    nc = bass.Bass("TRN2", target_bir_lowering=False)
    n_tiles = seq_len // T
    dt_in = lambda name, shape: nc.dram_tensor(name, shape, F32, kind="ExternalInput").ap()
    x_d = dt_in("x", [n_seq, seq_len, D])
    c_d = dt_in("c", [n_seq, D])
    ln1_d = dt_in("ln1_g", [L_TOTAL, D])
    ln2_d = dt_in("ln2_g", [L_TOTAL, D])
    wada_d = dt_in("w_ada", [L_TOTAL, D, 6 * D])
    bada_d = dt_in("b_ada", [L_TOTAL, 6 * D])
    win_d = dt_in("w_in", [L_TOTAL, D, IN_DIM])
    lbp_d = dt_in("lb_params", [L_TOTAL, 512])
    wgk_d = dt_in("w_gk", [L_TOTAL, 16, 256])
    bgk_d = dt_in("b_gk", [L_TOTAL, 256])
    gna_d = dt_in("gn_a", [L_TOTAL, 128])
    gnb_d = dt_in("gn_b", [L_TOTAL, 128])
    wout_d = dt_in("w_out", [L_TOTAL, D, D])
    wup_d = dt_in("w_up", [L_TOTAL, D, 2 * DFF])
    cw_d = dt_in("conv_w", [L_TOTAL, 3, 2 * DFF])
    cb_d = dt_in("conv_b", [L_TOTAL, 2 * DFF])
    wdn_d = dt_in("w_down", [L_TOTAL, DFF, D])
    lnf_d = dt_in("lnf_g", [D])
    ident_d = dt_in("ident", [128, 128])
    cmask_d = dt_in("cmask", [128, 512])
    rmask_d = dt_in("rmask", [128, 512])
    y_d = nc.dram_tensor("y", [n_seq, seq_len, D], F32, kind="ExternalOutput").ap()

    es = ExitStack()
    sb = lambda name, shape, dt: es.enter_context(nc.sbuf_tensor(name, shape, dt))
    X = sb("X", [128, KC, T], F32)
    H = sb("H", [128, KC, T], BF16)
    POOL = sb("POOL", [128, NPAGES, 512], F32)
    POOLB = POOL[:].bitcast(BF16)
    S = sb("S", [128, L_TOTAL, 6, 128], F32)
    HALO = sb("HALO", [128, L_TOTAL, 44, 2], F32)
    RING = sb("RING", [128, NSLOT, KC, 512], BF16)
    DEC = sb("DEC", [128, 6, 8], F32)
    IDF = sb("IDF", [128, 128], F32)
    IDB = sb("IDB", [128, 128], BF16)
    ONESB = sb("ONESB", [128, 128], BF16)
    CMASK = sb("CMASK", [128, 512], F32)
    RMASK = sb("RMASK", [128, 512], F32)
    G1 = sb("G1", [128, L_TOTAL * 8], F32)
    G2 = sb("G2", [128, L_TOTAL * 8], F32)
    LNF = sb("LNF", [128, 8], F32)
    NBGK = sb("NBGK", [128, L_TOTAL * 2], F32)
    GNA = sb("GNA", [128, L_TOTAL], F32)
    GNB = sb("GNB", [128, L_TOTAL], F32)
    CW = sb("CW", [128, L_TOTAL * 3 * 44], F32)
    CB = sb("CB", [128, L_TOTAL * 44], F32)
    LBP = sb("LBP", [128, L_TOTAL * 4], F32)
    OML = sb("OML", [128, L_TOTAL * 4], F32)
    BADA = sb("BADA", [128, L_TOTAL * 48], F32)
    CT = sb("CT", [128, n_seq * 8], F32)
    MOD = sb("MOD", [128, L_TOTAL, 48, n_seq], F32)
    GS1 = sb("GS1", [128, L_TOTAL, 8, n_seq], F32)
    GS2 = sb("GS2", [128, L_TOTAL, 8, n_seq], F32)
    WGK = sb("WGK", [16, L_TOTAL, 256], F32)
    CONST = sb("CONST", [128, 8], F32)
    SM = sb("SM", [128, 64], F32)
    ROWS = sb("ROWS", [128, 128], F32)
    HM = sb("HM", [128, 2], F32)
    STMP = sb("STMP", [128, 6, 128], F32)
    CORR = sb("CORR", [128, 44, 2], F32)
    CORT = sb("CORT", [128, 44], F32)
    PS = es.enter_context(nc.psum_tensor("PS", [128, 8, 512], F32))
    PSB = PS[:].bitcast(BF16)

    P = Prog(nc)
    bank_ctr = [0]
    ROT = [list(range(8))]

    def nb():
        r = ROT[0]
        b = r[bank_ctr[0] % len(r)]
        bank_ctr[0] += 1
        return b

    def run_tasks(tasks):
        tasks = [iter(t) for t in tasks]
        while tasks:
            for t in list(tasks):
                try:
                    next(t)
                except StopIteration:
                    tasks.remove(t)

    pk = lambda i: ('pg', i)
    pks = lambda i0, n: [('pg', i) for i in range(i0, i0 + n)]

    def b16(p0, c, n=1):
        page, half = p0 + c // 2, c % 2
        return POOLB[:, page, half * 512:half * 512 + 512 * n]

    P_QA, P_QB, P_KB, P_KA, P_SB16 = 0, 2, 3, 4, 4
    P_GT, P_OG = 10, 10
    P_RSTD, P_LNV, P_TMP0, P_TMP1 = 10, 11, 12, 13
    P_LF, P_BC, P_D1, P_D4, P_E0, P_E1 = 10, 11, 12, 13, 14, 15
    P_QT, P_SQ, P_KT, P_QH, P_KHT, P_KHM = 16, 16, 20, 23, 27, 28
    P_VA, P_VB, P_SGA, P_SGB, P_SCT = 34, 36, 38, 40, 42
    P_TMPS, P_OSB, P_SQH, P_RB = 46, 47, 48, 49
    P_XT = 16
    P_G, P_CV = 16, 0

    eps_ap = CONST[:, 0:1]
    one_ap = CONST[:, 1:2]
    ln8_ap = CONST[:, 2:3]

    P.op('dve', lambda e: e.memset(CONST[:, 0:1], EPS), writes=['CONST'])
    P.op('dve', lambda e: e.memset(CONST[:, 1:2], 1.0), writes=['CONST'])
    P.op('dve', lambda e: e.memset(CONST[:, 2:3], math.log(0.125)), writes=['CONST'])
    P.op('dve', lambda e: e.memset(CONST[:, 3:4], 0.0), writes=['CONST'])
    P.op('dve', lambda e: e.memset(CONST[:, 4:5], -QSHIFT), writes=['CONST'])
    P.op('dve', lambda e: e.memset(CONST[:, 5:6], math.log(0.125) - QSHIFT), writes=['CONST'])
    P.op('dve', lambda e: e.memset(ONESB[:], 1.0), writes=['ONESB'])
    P.op('dve', lambda e: e.memset(HM[:], 0.0), writes=['HM'])
    P.op('dve', lambda e: e.memset(HM[0:64, 0:1], 1.0), writes=['HM'])
    P.op('dve', lambda e: e.memset(HM[64:128, 1:2], 1.0), writes=['HM'])
    P.op('dve', lambda e: e.memset(POOL[:, 0:25, :], 0.0), writes=pks(0, 25))
    P.op('dve', lambda e: e.memset(POOL[:, 25:NPAGES, :], 0.0), writes=pks(25, NPAGES - 25))
    P.op('sp', lambda e: e.dma_start(out=IDF[:], in_=ident_d), writes=['IDF'], dma='c0')
    P.op('sp', lambda e: e.dma_start(out=CMASK[:], in_=cmask_d), writes=['CMASK'], dma='c1')
    P.op('sp', lambda e: e.dma_start(out=RMASK[:], in_=rmask_d), writes=['RMASK'], dma='c2')
    P.op('sp', lambda e: e.dma_start(out=WGK[:], in_=wgk_d.rearrange("l r k -> r l k")), writes=['WGK'], dma='c3')
    P.op('act', lambda e: e.activation(out=IDB[:], in_=IDF[:], func=AF.Copy), reads=['IDF'], writes=['IDB'])

    def load_rows_T(dst, dst_key, src_rows, R):
        P.op('sp', lambda e: e.dma_start(out=ROWS[0:R, :], in_=src_rows), writes=['ROWS'], dma='rows')
        b = nb()
        P.op('pe', lambda e: e.transpose(out=PS[:, b, 0:R], in_=ROWS[0:R, :], identity=IDF[0:R, 0:R]),
             reads=['ROWS', 'IDF'], writes=[('ps', b)])
        P.op('act', lambda e: e.activation(out=dst, in_=PS[:, b, 0:R], func=AF.Copy),
             reads=[('ps', b)], writes=[dst_key])

    def load_param(dst_tile, key, src2d, total_rows):
        r0 = 0
        while r0 < total_rows:
            R = min(128, total_rows - r0)
            load_rows_T(dst_tile[:, r0:r0 + R], key, src2d[r0:r0 + R, :], R)
            r0 += R

    load_param(G1, 'G1', ln1_d.rearrange("l (c p) -> (l c) p", p=128), L_TOTAL * 8)
    load_param(G2, 'G2', ln2_d.rearrange("l (c p) -> (l c) p", p=128), L_TOTAL * 8)
    load_param(LNF, 'LNF', lnf_d.rearrange("(c p) -> c p", p=128), 8)
    load_param(NBGK, 'NBGK', bgk_d.rearrange("l (c p) -> (l c) p", p=128), L_TOTAL * 2)
    load_param(GNA, 'GNA', gna_d, L_TOTAL)
    load_param(GNB, 'GNB', gnb_d, L_TOTAL)
    load_param(CW, 'CW', cw_d.rearrange("l j (m p) -> (l j m) p", p=128), L_TOTAL * 3 * 44)
    load_param(CB, 'CB', cb_d.rearrange("l (m p) -> (l m) p", p=128), L_TOTAL * 44)
    load_param(LBP, 'LBP', lbp_d.rearrange("l (c p) -> (l c) p", p=128), L_TOTAL * 4)
    load_param(BADA, 'BADA', bada_d.rearrange("l (c p) -> (l c) p", p=128), L_TOTAL * 48)
    load_param(CT, 'CT', c_d.rearrange("s (c p) -> (s c) p", p=128), n_seq * 8)
    P.op('dve', lambda e: e.tensor_scalar(out=NBGK[:], in0=NBGK[:], scalar1=-1.0, scalar2=None, op0=ALU.mult),
         reads=['NBGK'], writes=['NBGK'])
    P.op('act', lambda e: e.activation(out=CT[:], in_=CT[:], func=AF.Silu), reads=['CT'], writes=['CT'])
    lb4 = LBP[:].rearrange("p (l c) -> p l c", c=4)
    mx, ex, sm_, rs_ = SM[:, 0:4], SM[:, 4:20].rearrange("p (l c) -> p l c", c=4), SM[:, 20:24], SM[:, 24:28]
    P.op('dve', lambda e: e.tensor_tensor(out=mx, in0=lb4[:, 0, :], in1=lb4[:, 1, :], op=ALU.max), reads=['LBP'], writes=['SM'])
    P.op('dve', lambda e: e.tensor_tensor(out=mx, in0=mx, in1=lb4[:, 2, :], op=ALU.max), reads=['LBP', 'SM'], writes=['SM'])
    P.op('dve', lambda e: e.tensor_tensor(out=mx, in0=mx, in1=lb4[:, 3, :], op=ALU.max), reads=['LBP', 'SM'], writes=['SM'])
    for l in range(4):
        P.op('dve', lambda e, l=l: e.tensor_tensor(out=ex[:, l, :], in0=lb4[:, l, :], in1=mx, op=ALU.subtract),
             reads=['LBP', 'SM'], writes=['SM'])
    P.op('act', lambda e: e.activation(out=SM[:, 4:20], in_=SM[:, 4:20], func=AF.Exp), reads=['SM'], writes=['SM'])
    P.op('dve', lambda e: e.tensor_tensor(out=sm_, in0=ex[:, 0, :], in1=ex[:, 1, :], op=ALU.add), reads=['SM'], writes=['SM'])
    P.op('dve', lambda e: e.tensor_tensor(out=sm_, in0=sm_, in1=ex[:, 2, :], op=ALU.add), reads=['SM'], writes=['SM'])
    P.op('dve', lambda e: e.tensor_tensor(out=sm_, in0=sm_, in1=ex[:, 3, :], op=ALU.add), reads=['SM'], writes=['SM'])
    P.op('dve', lambda e: e.reciprocal(out=rs_, in_=sm_), reads=['SM'], writes=['SM'])
    oml4 = OML[:].rearrange("p (l c) -> p l c", c=4)
    P.op('dve', lambda e: e.memset(OML[:], 1.0), writes=['OML'])
    for l in range(1, 4):
        P.op('dve', lambda e, l=l: e.tensor_tensor(out=ex[:, l, :], in0=ex[:, l, :], in1=rs_, op=ALU.mult),
             reads=['SM'], writes=['SM'])
        P.op('dve', lambda e, l=l: e.tensor_tensor(out=oml4[:, l, :], in0=oml4[:, l - 1, :], in1=ex[:, l, :], op=ALU.subtract),
             reads=['SM', 'OML'], writes=['OML'])

    ct3 = CT[:].rearrange("p (s c) -> p s c", c=8)
    wa_bufs = [(POOL[:, 0:12, :], pks(0, 12)), (POOL[:, 12:24, :], pks(12, 12))]
    wa_i = 0
    for l in layers:
        b = nb()
        for q in range(8):
            buf, bkeys = wa_bufs[wa_i % 2]
            wa_i += 1
            bufv = buf.rearrange("p a n -> p (a n)").rearrange("p (k n) -> p k n", k=8)
            src = wada_d[l].rearrange("(k p) n -> p k n", p=128)[:, :, q * 768:(q + 1) * 768]
            P.op('sp', lambda e, bufv=bufv, src=src: e.dma_start(out=bufv, in_=src), writes=bkeys, dma='wa%d' % (wa_i % 2))
            for j6 in range(6):
                jc = q * 6 + j6
                for kc in range(8):
                    P.op('pe', lambda e, bufv=bufv, j6=j6, jc=jc, kc=kc, b=b: e.matmul(
                        PS[:, b, jc * n_seq:(jc + 1) * n_seq], lhsT=bufv[:, kc, j6 * 128:(j6 + 1) * 128],
                        rhs=ct3[:, :, kc], start=(kc == 0), stop=(kc == 7)),
                        reads=bkeys + ['CT'], writes=[('ps', b)])
        bada3 = BADA[:, l * 48:(l + 1) * 48]
        P.op('dve', lambda e, l=l, b=b, bada3=bada3: e.tensor_tensor(
            out=MOD[:, l, :, :], in0=PS[:, b, 0:48 * n_seq].rearrange("p (j s) -> p j s", s=n_seq),
            in1=bada3.unsqueeze(2).to_broadcast([128, 48, n_seq]), op=ALU.add),
            reads=[('ps', b), 'BADA'], writes=['MOD'])
        for (GS, Gp, joff) in ((GS1, G1, 1), (GS2, G2, 4)):
            P.op('dve', lambda e, l=l, GS=GS, joff=joff: e.tensor_scalar(
                out=GS[:, l, :, :], in0=MOD[:, l, joff * 8:(joff + 1) * 8, :], scalar1=1.0, scalar2=None, op0=ALU.add),
                reads=['MOD'], writes=['GS'])
            P.op('dve', lambda e, l=l, GS=GS, Gp=Gp: e.tensor_tensor(
                out=GS[:, l, :, :], in0=GS[:, l, :, :],
                in1=Gp[:, l * 8:(l + 1) * 8].unsqueeze(2).to_broadcast([128, 8, n_seq]), op=ALU.mult),
                reads=['GS', 'G1', 'G2'], writes=['GS'])
    P.op('dve', lambda e: e.memset(POOL[:, 0:24, :], 0.0), writes=pks(0, 24))

    wloads = []
    WSCR = nc.dram_tensor("wscr", [len(layers), 27, 128, KC * 512], BF16, kind="Internal").ap()

    def plan_weights():
        for s in range(n_seq):
            for t in range(n_tiles):
                for li, l in enumerate(layers):
                    first = (s == 0 and t == 0)
                    ent = []
                    wv = win_d[l].rearrange("(k p) n -> p k n", p=128)
                    for ci in (1, 0, 7, 4, 2, 5, 3, 6):
                        if ci == 7:
                            ent.append([(lambda sl: RING[:, sl, :, 0:16], wv[:, :, 3584:3600])])
                        else:
                            ent.append([(lambda sl: RING[:, sl, :, :], wv[:, :, ci * 512:(ci + 1) * 512])])
                    ov = wout_d[l].rearrange("(k p) n -> p k n", p=128)
                    for ci in range(2):
                        ent.append([(lambda sl: RING[:, sl, :, :], ov[:, :, ci * 512:(ci + 1) * 512])])
                    uv = wup_d[l].rearrange("(k p) n -> p k n", p=128)
                    for i in range(11):
                        ent.append([
                            (lambda sl: RING[:, sl, :, 0:256], uv[:, :, 256 * i:256 * i + 256]),
                            (lambda sl: RING[:, sl, :, 256:512], uv[:, :, DFF + 256 * i:DFF + 256 * i + 256])])
                    dv = wdn_d[l].rearrange("(k p) n -> p k n", p=128)
                    for hf in range(2):
                        for g3 in range(3):
                            k0 = g3 * 8
                            k1 = min(22, k0 + 8)
                            ent.append([(lambda sl, k0=k0, k1=k1: RING[:, sl, 0:k1 - k0, :],
                                         dv[:, k0:k1, hf * 512:(hf + 1) * 512])])
                    assert len(ent) == 27
                    for idx, pieces in enumerate(ent):
                        wloads.append((pieces, li, idx, first))
    plan_weights()
    wstate = {'issued': 0, 'consumed': 0}

    def issue_load():
        i = wstate['issued']
        if i >= len(wloads):
            return
        sl = i % NSLOT
        pieces, li, idx, first = wloads[i]
        flat = RING[:, sl, :, :].rearrange("p k n -> p (k n)")
        if first:
            for (dstf, src) in pieces:
                dst = dstf(sl)
                P.op('pool', lambda e, dst=dst, src=src: e.dma_start(out=dst, in_=src), writes=[('W', sl)], dma='w%d' % sl)
            if n_seq * n_tiles > 1:
                P.op('sp', lambda e, flat=flat, li=li, idx=idx: e.dma_start(out=WSCR[li, idx], in_=flat),
                     reads=[('W', sl)], writes=[('wscr', li, idx)], dma='wb%d' % sl)
        else:
            P.op('sp', lambda e, flat=flat, li=li, idx=idx: e.dma_start(out=flat, in_=WSCR[li, idx]),
                 reads=[('wscr', li, idx)], writes=[('W', sl)], dma='w%d' % sl)
        wstate['issued'] += 1

    def next_w():
        i = wstate['consumed']
        wstate['consumed'] += 1
        return i % NSLOT

    def release_w():
        issue_load()

    for _ in range(NSLOT):
        issue_load()

    Hkeys = [('H', k) for k in range(8)]
    Xkeys = [('X', k) for k in range(8)]

    def norm_mod(src_keys_fn, l, s, GS, shoff):
        for dc in range(8):
            P.op('act', lambda e, dc=dc: e.activation(out=b16(P_SQ, dc), in_=X[:, dc, :], func=AF.Square),
                 reads=[('X', dc)], writes=[pk(P_SQ + dc // 2)])
        b = nb()
        for dc in range(8):
            P.op('pe', lambda e, dc=dc, b=b: e.matmul(PS[:, b, :], lhsT=ONESB[:], rhs=b16(P_SQ, dc),
                                                     start=(dc == 0), stop=(dc == 7)),
                 reads=[pk(P_SQ + dc // 2), 'ONESB'], writes=[('ps', b)])
        P.op('act', lambda e, b=b: e.activation(out=POOL[:, P_LNV, :], in_=PS[:, b, :], func=AF.Ln, scale=1.0 / D, bias=eps_ap),
             reads=[('ps', b), 'CONST'], writes=[pk(P_LNV)])
        P.op('act', lambda e: e.activation(out=POOL[:, P_RSTD, :], in_=POOL[:, P_LNV, :], func=AF.Exp, scale=-0.5),
             reads=[pk(P_LNV)], writes=[pk(P_RSTD)])
        for dc in range(8):
            tp = P_TMP0 + (dc % 2)
            P.op('dve', lambda e, dc=dc, tp=tp: e.tensor_tensor(out=POOL[:, tp, :], in0=X[:, dc, :], in1=POOL[:, P_RSTD, :], op=ALU.mult),
                 reads=[('X', dc), pk(P_RSTD)], writes=[pk(tp)])
            P.op('act', lambda e, dc=dc, tp=tp: e.activation(out=H[:, dc, :], in_=POOL[:, tp, :], func=AF.Identity,
                                                            scale=GS[:, l, dc, s:s + 1], bias=MOD[:, l, shoff * 8 + dc, s:s + 1]),
                 reads=[pk(tp), 'GS', 'MOD'], writes=[('H', dc)])

    def mm_fm(sl, col0, ncols_chunks, evac):
        for ci in range(ncols_chunks):
            b = nb()
            for kc in range(8):
                P.op('pe', lambda e, ci=ci, kc=kc, b=b: e.matmul(
                    PS[:, b, :], lhsT=RING[:, sl, kc, col0 + ci * 128:col0 + (ci + 1) * 128], rhs=H[:, kc, :],
                    start=(kc == 0), stop=(kc == 7)),
                    reads=[('W', sl), ('H', kc)], writes=[('ps', b)])
            evac(ci, b)
            yield

    def mm_tm(sl, dst_page):
        for blk in range(NBLK):
            b = nb()
            for kc in range(8):
                P.op('pe', lambda e, kc=kc, b=b, blk=blk: e.matmul(
                    PS[:, b, :], lhsT=H[:, kc, blk * 128:(blk + 1) * 128], rhs=RING[:, sl, kc, :],
                    start=(kc == 0), stop=(kc == 7)),
                    reads=[('W', sl), ('H', kc)], writes=[('ps', b)])
            P.op('act', lambda e, b=b, blk=blk: e.activation(out=b16(dst_page, blk), in_=PS[:, b, :], func=AF.Copy),
                 reads=[('ps', b)], writes=[pk(dst_page + blk // 2)])
            yield

    TSETS = [dict(LF=10, BC=11, D1=12, D4=13, E=[14, 15, 8, 9]), dict(LF=42, BC=43, D1=44, D4=45, E=[46, 47, 48, 50])]

    def gate_chunk(fc, l, ts, gflags):
        T_LF, T_BC, T_D1, T_D4 = ts['LF'], ts['BC'], ts['D1'], ts['D4']
        T_E = ts['E']
        if fc < 4:
            hc = fc
            sigma, qb_ap, qt_ap = 1.0, CONST[:, 3:4], CONST[:, 4:5]
            q_ap, k_ap = b16(P_QA, hc), POOL[:, P_KA + hc, :]
            qk_keys = [pk(P_QA + hc // 2), pk(P_KA + hc)]
            P.op('dve', lambda e: e.tensor_scalar(out=POOL[:, P_KA + hc, :], in0=POOL[:, P_KA + hc, :],
                                                  scalar1=OML[:, l * 4 + hc:l * 4 + hc + 1], scalar2=None, op0=ALU.mult),
                 reads=[pk(P_KA + hc), 'OML'], writes=[pk(P_KA + hc)])
            P.op('act', lambda e: e.activation(out=POOL[:, T_LF, :], in_=POOL[:, P_KA + hc, :], func=AF.Ln, scale=-1.0, bias=one_ap),
                 reads=[pk(P_KA + hc), 'CONST'], writes=[pk(T_LF)])
        else:
            c2 = fc - 4
            sigma, qb_ap, qt_ap = -1.0 / 16.0, ln8_ap, CONST[:, 5:6]
            q_ap, k_ap = b16(P_QB, c2), b16(P_KB, c2)
            qk_keys = [pk(P_QB), pk(P_KB)]
            bg = nb()
            P.op('pe', lambda e: e.matmul(PS[:, bg, :], lhsT=WGK[0:16, l, c2 * 128:(c2 + 1) * 128], rhs=POOL[0:16, P_RB, :],
                                          start=True, stop=True),
                 reads=[pk(P_RB), 'WGK'], writes=[('ps', bg)])
            P.op('act', lambda e: e.activation(out=POOL[:, T_E[0], :], in_=PS[:, bg, :], func=AF.Exp, scale=-1.0,
                                               bias=NBGK[:, l * 2 + c2:l * 2 + c2 + 1]),
                 reads=[('ps', bg), 'NBGK'], writes=[pk(T_E[0])])
            P.op('act', lambda e: e.activation(out=POOL[:, T_LF, :], in_=POOL[:, T_E[0], :], func=AF.Ln, bias=one_ap),
                 reads=[pk(T_E[0]), 'CONST'], writes=[pk(T_LF)])
        yield
        bc3 = POOL[:, T_BC, :].rearrange("p (c t) -> p c t", t=64)
        P.op('dve', lambda e: e.tensor_tensor_scan(out=POOL[:, T_BC, :], data0=RMASK[:], data1=POOL[:, T_LF, :],
                                                   initial=0.0, op0=ALU.mult, op1=ALU.add),
             reads=[pk(T_LF), 'RMASK'], writes=[pk(T_BC)])
        yield
        P.op('dve', lambda e: e.tensor_tensor(out=POOL[:, T_D1, :].rearrange("p (c t) -> p c t", t=64), in0=bc3,
                                              in1=bc3[:, :, 32:33].to_broadcast([128, 8, 64]), op=ALU.subtract),
             reads=[pk(T_BC)], writes=[pk(T_D1)])
        P.op('act', lambda e: e.activation(out=DEC[:, fc, :], in_=POOL[:, T_BC, 63:512:64], func=AF.Exp, scale=sigma),
             reads=[pk(T_BC)], writes=[('DEC', fc)])
        yield
        P.op('dve', lambda e: e.tensor_tensor(out=POOL[:, T_D4, :].rearrange("p (c t) -> p c t", t=64),
                                              in0=bc3[:, :, 63:64].to_broadcast([128, 8, 64]), in1=bc3, op=ALU.subtract),
             reads=[pk(T_BC)], writes=[pk(T_D4)])
        yield
        plan = [
            (T_D4, sigma, CONST[:, 3:4], 'k', 'KHT'),
            (T_D1, -sigma, CONST[:, 3:4], 'k', 'KT'),
            (T_D1, sigma, qt_ap, 'q', 'QT'),
            (T_BC, sigma, qb_ap, 'q', 'QH'),
        ]
        for i, (src, sc, bias, which, kind) in enumerate(plan):
            if which == 'q' and fc < 4:
                while not gflags.get('qa'):
                    yield
            ep = T_E[i]
            P.op('act', lambda e, src=src, sc=sc, bias=bias, ep=ep: e.activation(
                out=POOL[:, ep, :], in_=POOL[:, src, :], func=AF.Exp, scale=sc, bias=bias),
                reads=[pk(src), 'CONST'], writes=[pk(ep)])
            yield
            src_ap = q_ap if which == 'q' else k_ap
            if kind in ('QT', 'QH'):
                p0 = P_QT if kind == 'QT' else P_QH
                if fc < 4:
                    P.op('dve', lambda e, ep=ep, src_ap=src_ap, p0=p0: e.tensor_tensor(
                        out=b16(p0, fc), in0=src_ap, in1=POOL[:, ep, :], op=ALU.mult),
                        reads=[pk(ep)] + qk_keys, writes=[pk(p0 + fc // 2)])
                else:
                    for hh in range(2):
                        g = (fc - 4) * 2 + hh
                        P.op('dve', lambda e, ep=ep, src_ap=src_ap, p0=p0, g=g, hh=hh: e.scalar_tensor_tensor(
                            out=b16(p0, 4 + g), in0=src_ap, scalar=HM[:, hh:hh + 1], in1=POOL[:, ep, :],
                            op0=ALU.mult, op1=ALU.mult),
                            reads=[pk(ep), 'HM'] + qk_keys, writes=[pk(p0 + (4 + g) // 2)])
            elif kind == 'KT':
                P.op('dve', lambda e, ep=ep, src_ap=src_ap: e.tensor_tensor(
                    out=b16(P_KT, fc), in0=src_ap, in1=POOL[:, ep, :], op=ALU.mult),
                    reads=[pk(ep)] + qk_keys, writes=[pk(P_KT + fc // 2)])
            else:
                kht = b16(P_KHT, fc % 2)
                P.op('dve', lambda e, ep=ep, src_ap=src_ap, kht=kht: e.tensor_tensor(
                    out=kht, in0=src_ap, in1=POOL[:, ep, :], op=ALU.mult),
                    reads=[pk(ep)] + qk_keys, writes=[pk(P_KHT)])
                yield
                b = nb()
                for blk in range(NBLK):
                    P.op('pe', lambda e, b=b, blk=blk, kht=kht: e.transpose(
                        out=PSB[:, b, blk * 128:(blk + 1) * 128], in_=kht[:, blk * 128:(blk + 1) * 128], identity=IDB[:]),
                        reads=[pk(P_KHT), 'IDB'], writes=[('ps', b)])
                khm = POOLB[:, P_KHM + fc, :].rearrange("p (j d) -> p j d", d=128)
                pv = PSB[:, b, 0:512].rearrange("p (k d) -> p k d", d=128)
                P.op('act', lambda e, khm=khm, pv=pv, b=b: e.activation(out=khm[0:64, 0:8:2, :], in_=pv[0:64, :, :], func=AF.Copy),
                     reads=[('ps', b)], writes=[pk(P_KHM + fc)])
                P.op('act', lambda e, khm=khm, pv=pv, b=b: e.activation(out=khm[64:128, 1:8:2, :], in_=pv[64:128, :, :], func=AF.Copy),
                     reads=[('ps', b)], writes=[pk(P_KHM + fc)])
            yield

    def layer_step(l, s, t, first_tile):
        norm_mod(None, l, s, GS1, 0)
        flags = {}

        def ev_qa(ci, b):
            P.op('act', lambda e: e.activation(out=b16(P_QA, ci), in_=PS[:, b, :], func=AF.Copy),
                 reads=[('ps', b)], writes=[pk(P_QA + ci // 2)])

        def ev_fa(ci, b):
            P.op('act', lambda e: e.activation(out=POOL[:, P_KA + ci, :], in_=PS[:, b, :], func=AF.Sigmoid, scale=-1.0),
                 reads=[('ps', b)], writes=[pk(P_KA + ci)])

        def ev_sga(ci, b):
            P.op('act', lambda e: e.activation(out=b16(P_SGA, ci), in_=PS[:, b, :], func=AF.Copy),
                 reads=[('ps', b)], writes=[pk(P_SGA + ci // 2)])

        def ev_sgb(ci, b):
            P.op('act', lambda e: e.activation(out=b16(P_SGB, ci), in_=PS[:, b, :], func=AF.Copy),
                 reads=[('ps', b)], writes=[pk(P_SGB + ci // 2)])

        def ev_qk(ci, b):
            if ci < 2:
                P.op('act', lambda e: e.activation(out=b16(P_QB, ci), in_=PS[:, b, :], func=AF.Copy),
                     reads=[('ps', b)], writes=[pk(P_QB)])
            else:
                P.op('act', lambda e: e.activation(out=b16(P_KB, ci - 2), in_=PS[:, b, :], func=AF.Copy),
                     reads=[('ps', b)], writes=[pk(P_KB)])

        sl = next_w()
        for _ in mm_fm(sl, 0, 4, ev_fa):
            pass
        release_w()
        def main_task():
            sl = next_w()
            yield from mm_fm(sl, 0, 4, ev_qa)
            release_w()
            flags['qa'] = True
            sl = next_w()
            b = nb()
            for kc in range(8):
                P.op('pe', lambda e, kc=kc, b=b, sl=sl: e.matmul(PS[0:16, b, :], lhsT=RING[:, sl, kc, 0:16], rhs=H[:, kc, :],
                                                                start=(kc == 0), stop=(kc == 7)),
                     reads=[('W', sl), ('H', kc)], writes=[('ps', b)])
            P.op('act', lambda e, b=b: e.activation(out=POOL[0:16, P_RB, :], in_=PS[0:16, b, :], func=AF.Copy),
                 reads=[('ps', b)], writes=[pk(P_RB)])
            release_w()
            yield
            sl = next_w()
            yield from mm_fm(sl, 0, 4, ev_qk)
            release_w()
            flags['gla'] = True
            sl = next_w()
            yield from mm_tm(sl, P_VA)
            release_w()
            sl = next_w()
            yield from mm_tm(sl, P_VB)
            release_w()
            sl = next_w()
            yield from mm_fm(sl, 0, 4, ev_sga)
            release_w()
            sl = next_w()
            yield from mm_fm(sl, 0, 4, ev_sgb)
            release_w()

        def gate_task(fcs, ts):
            for fc in fcs:
                if fc >= 4:
                    while not flags.get('gla'):
                        yield
                yield from gate_chunk(fc, l, ts, flags)

        run_tasks([main_task(), gate_task([0, 2, 4], TSETS[0]), gate_task([1, 3, 5], TSETS[1])])

        def head_info(h):
            if h < 4:
                return dict(fc=h, qt=b16(P_QT, h), qtk=pk(P_QT + h // 2), kt=b16(P_KT, h), ktk=pk(P_KT + h // 2),
                            qh=b16(P_QH, h), qhk=pk(P_QH + h // 2), vpage=P_VA, vcol=h * 128,
                            gn=GNA[:, l:l + 1], sg=b16(P_SGA, h), sgk=pk(P_SGA + h // 2))
            g = h - 4
            fc = 4 + g // 2
            return dict(fc=fc, qt=b16(P_QT, 4 + g), qtk=pk(P_QT + (4 + g) // 2), kt=b16(P_KT, fc), ktk=pk(P_KT + fc // 2),
                        qh=b16(P_QH, 4 + g), qhk=pk(P_QH + (4 + g) // 2), vpage=P_VB, vcol=g * 128,
                        gn=GNB[:, l:l + 1], sg=b16(P_SGB, g), sgk=pk(P_SGB + g // 2))

        def scores_task():
            for h in range(8):
                hi = head_info(h)
                b = nb()
                for blk in range(NBLK):
                    P.op('pe', lambda e, hi=hi, b=b, blk=blk: e.matmul(
                        PS[:, b, blk * 128:(blk + 1) * 128], lhsT=hi['kt'][:, blk * 128:(blk + 1) * 128],
                        rhs=hi['qt'][:, blk * 128:(blk + 1) * 128], start=True, stop=True),
                        reads=[hi['ktk'], hi['qtk']], writes=[('ps', b)])
                P.op('dve', lambda e, b=b: e.tensor_scalar(out=POOL[:, P_TMPS, :], in0=PS[:, b, :], scalar1=1e30, scalar2=-1e30,
                                                           op0=ALU.min, op1=ALU.max),
                     reads=[('ps', b)], writes=[pk(P_TMPS)])
                P.op('pool', lambda e, h=h: e.tensor_tensor(out=b16(P_SCT, h), in0=POOL[:, P_TMPS, :], in1=CMASK[:], op=ALU.mult),
                     reads=[pk(P_TMPS), 'CMASK'], writes=[pk(P_SCT + h // 2)])
                yield

        def state_task(fcs, banks):
            for i, fc in enumerate(fcs):
                skey = ('S', l, fc)
                if first_tile:
                    P.op('dve', lambda e, fc=fc: e.memset(S[:, l, fc, :], 0.0), writes=[skey])
                for j in range(NCH):
                    bnk = banks[2 * i + (j % 2)]
                    reg = (j // 2) * 128
                    if fc < 4:
                        P.op('pe', lambda e, fc=fc, j=j, bnk=bnk, reg=reg: e.matmul(
                            PS[:, bnk, reg:reg + 128], lhsT=POOLB[:, P_KHM + fc, j * 128:(j + 1) * 128],
                            rhs=b16(P_VA, j // 2)[:, fc * 128:(fc + 1) * 128], start=True, stop=True),
                            reads=[pk(P_KHM + fc), pk(P_VA + (j // 2) // 2)], writes=[('ps', bnk)])
                    else:
                        for hh in range(2):
                            g = (fc - 4) * 2 + hh
                            P.op('pe', lambda e, fc=fc, j=j, bnk=bnk, reg=reg, hh=hh, g=g: e.matmul(
                                PS[hh * 64:(hh + 1) * 64, bnk, reg:reg + 128],
                                lhsT=POOLB[:, P_KHM + fc, j * 128 + hh * 64:j * 128 + (hh + 1) * 64],
                                rhs=b16(P_VB, j // 2)[:, g * 128:(g + 1) * 128], start=True, stop=True),
                                reads=[pk(P_KHM + fc), pk(P_VB + (j // 2) // 2)], writes=[('ps', bnk)])
                yield
            for j in range(NCH):
                for i, fc in enumerate(fcs):
                    sb16 = POOLB[:, P_SB16 + fc, :].rearrange("p (j e) -> p j e", e=128)
                    bnk = banks[2 * i + (j % 2)]
                    reg = (j // 2) * 128
                    if j % 2 == 0:
                        src, skey, dst, dkey = S[:, l, fc, :], ('S', l, fc), STMP[:, fc, :], ('STMP', fc)
                    else:
                        src, skey, dst, dkey = STMP[:, fc, :], ('STMP', fc), S[:, l, fc, :], ('S', l, fc)
                    P.op('act', lambda e, j=j, sb16=sb16, src=src: e.activation(out=sb16[:, j, :], in_=src, func=AF.Copy),
                         reads=[skey], writes=[pk(P_SB16 + fc)])
                    P.op('dve', lambda e, fc=fc, j=j, bnk=bnk, reg=reg, src=src, dst=dst: e.scalar_tensor_tensor(
                        out=dst, in0=src, scalar=DEC[:, fc, j:j + 1], in1=PS[:, bnk, reg:reg + 128],
                        op0=ALU.mult, op1=ALU.add),
                        reads=[skey, ('DEC', fc), ('ps', bnk)], writes=[dkey])
                yield

        def o_task(heads, tmps=P_TMPS, osb=P_OSB, sqh=0):
            for h in heads:
                hi = head_info(h)
                fc = hi['fc']
                sb16 = POOLB[:, P_SB16 + fc, :].rearrange("p (j e) -> p j e", e=128)
                b = nb()
                for blk in range(NBLK):
                    P.op('pe', lambda e, hi=hi, b=b, blk=blk, h=h: e.matmul(
                        PS[:, b, blk * 128:(blk + 1) * 128], lhsT=b16(hi['vpage'], blk)[:, hi['vcol']:hi['vcol'] + 128],
                        rhs=b16(P_SCT, h)[:, blk * 128:(blk + 1) * 128], start=True, stop=False),
                        reads=[pk(hi['vpage'] + blk // 2), pk(P_SCT + h // 2)], writes=[('ps', b)])
                    for jj in range(2):
                        j = blk * 2 + jj
                        P.op('pe', lambda e, hi=hi, b=b, j=j, sb16=sb16, jj=jj: e.matmul(
                            PS[:, b, j * 64:(j + 1) * 64], lhsT=sb16[:, j, :], rhs=hi['qh'][:, j * 64:(j + 1) * 64],
                            start=False, stop=(jj == 1)),
                            reads=[pk(P_SB16 + fc), hi['qhk']], writes=[('ps', b)])
                yield
                P.op('act', lambda e, b=b: e.activation(out=b16(P_SQH, sqh), in_=PS[:, b, :], func=AF.Square),
                     reads=[('ps', b)], writes=[pk(P_SQH)])
                b2 = nb()
                P.op('pe', lambda e, b2=b2: e.matmul(PS[:, b2, :], lhsT=ONESB[:], rhs=b16(P_SQH, sqh), start=True, stop=True),
                     reads=[pk(P_SQH), 'ONESB'], writes=[('ps', b2)])
                yield
                P.op('act', lambda e, b2=b2: e.activation(out=POOL[:, tmps, :], in_=PS[:, b2, :], func=AF.Ln, scale=1.0 / 128.0, bias=eps_ap),
                     reads=[('ps', b2), 'CONST'], writes=[pk(tmps)])
                yield
                P.op('act', lambda e: e.activation(out=POOL[:, tmps, :], in_=POOL[:, tmps, :], func=AF.Exp, scale=-0.5),
                     reads=[pk(tmps)], writes=[pk(tmps)])
                yield
                P.op('dve', lambda e, hi=hi, b=b: e.scalar_tensor_tensor(out=POOL[:, osb, :], in0=PS[:, b, :], scalar=hi['gn'],
                                                                        in1=POOL[:, tmps, :], op0=ALU.mult, op1=ALU.mult),
                     reads=[('ps', b), pk(tmps), 'GNA', 'GNB'], writes=[pk(osb)])
                yield
                P.op('pool', lambda e, hi=hi, h=h: e.tensor_tensor(out=b16(P_OG, h), in0=POOL[:, osb, :], in1=hi['sg'], op=ALU.mult),
                     reads=[pk(osb), hi['sgk']], writes=[pk(P_OG + h // 2)])
                yield

        def silu_task():
            for pg in (P_SGA, P_SGB):
                for ci in range(4):
                    P.op('act', lambda e, pg=pg, ci=ci: e.activation(out=b16(pg, ci), in_=b16(pg, ci), func=AF.Silu),
                         reads=[pk(pg + ci // 2)], writes=[pk(pg + ci // 2)])
                    yield

        ROT[0] = [6, 7]
        run_tasks([scores_task(), state_task([0, 1, 2], [0, 1, 2, 3, 4, 5]), silu_task()])
        run_tasks([state_task([3, 4, 5], [0, 1, 2, 3, 4, 5]), o_task([0, 1, 2])])
        ROT[0] = list(range(8))
        run_tasks([o_task([3, 5, 7]), o_task([4, 6], tmps=14, osb=15, sqh=1)])

        slots = [next_w(), next_w()]
        for n in range(8):
            sl = slots[n // 4]
            b = nb()
            for kc in range(8):
                P.op('pe', lambda e, n=n, kc=kc, b=b, sl=sl: e.matmul(
                    PS[:, b, :], lhsT=RING[:, sl, kc, (n % 4) * 128:(n % 4 + 1) * 128], rhs=b16(P_OG, kc),
                    start=(kc == 0), stop=(kc == 7)),
                    reads=[('W', sl), pk(P_OG + kc // 2)], writes=[('ps', b)])
            P.op('dve', lambda e, n=n, b=b: e.scalar_tensor_tensor(
                out=X[:, n, :], in0=PS[:, b, :], scalar=MOD[:, l, 2 * 8 + n, s:s + 1], in1=X[:, n, :], op0=ALU.mult, op1=ALU.add),
                reads=[('ps', b), ('X', n), 'MOD'], writes=[('X', n)])
            if n == 3:
                release_w()
        release_w()

        norm_mod(None, l, s, GS2, 3)

        hkey = ('HALO', l)
        if first_tile:
            P.op('dve', lambda e: e.memset(HALO[:, l, :, :], 0.0), writes=[hkey])
        W0 = CW[:, (l * 3 + 0) * 44:(l * 3 + 0) * 44 + 44]
        W1 = CW[:, (l * 3 + 1) * 44:(l * 3 + 1) * 44 + 44]
        P.op('dve', lambda e: e.tensor_tensor(out=CORR[:, :, 1], in0=W0, in1=HALO[:, l, :, 1], op=ALU.mult),
             reads=[hkey, 'CW'], writes=['CORR'])
        P.op('dve', lambda e: e.tensor_tensor(out=CORR[:, :, 0], in0=W0, in1=HALO[:, l, :, 0], op=ALU.mult),
             reads=[hkey, 'CW'], writes=['CORR'])
        P.op('dve', lambda e: e.tensor_tensor(out=CORT[:], in0=W1, in1=HALO[:, l, :, 1], op=ALU.mult),
             reads=[hkey, 'CW'], writes=['CORT'])
        P.op('dve', lambda e: e.tensor_tensor(out=CORR[:, :, 0], in0=CORR[:, :, 0], in1=CORT[:], op=ALU.add),
             reads=['CORR', 'CORT'], writes=['CORR'])

        def conv(b, m, ypage):
            w0 = CW[:, (l * 3 + 0) * 44 + m:(l * 3 + 0) * 44 + m + 1]
            w1 = CW[:, (l * 3 + 1) * 44 + m:(l * 3 + 1) * 44 + m + 1]
            w2 = CW[:, (l * 3 + 2) * 44 + m:(l * 3 + 2) * 44 + m + 1]
            cb = CB[:, l * 44 + m:l * 44 + m + 1]
            Y = POOL[:, ypage, :]
            P.op('act', lambda e: e.activation(out=Y, in_=PS[:, b, :], func=AF.Identity, scale=w2, bias=cb),
                 reads=[('ps', b), 'CW', 'CB'], writes=[pk(ypage)])
            P.op('dve', lambda e: e.scalar_tensor_tensor(out=Y[:, 1:512], in0=PS[:, b, 0:511], scalar=w1, in1=Y[:, 1:512],
                                                         op0=ALU.mult, op1=ALU.add),
                 reads=[('ps', b), pk(ypage), 'CW'], writes=[pk(ypage)])
            P.op('dve', lambda e: e.scalar_tensor_tensor(out=Y[:, 2:512], in0=PS[:, b, 0:510], scalar=w0, in1=Y[:, 2:512],
                                                         op0=ALU.mult, op1=ALU.add),
                 reads=[('ps', b), pk(ypage), 'CW'], writes=[pk(ypage)])
            P.op('dve', lambda e: e.tensor_tensor(out=Y[:, 0:2], in0=Y[:, 0:2], in1=CORR[:, m, :], op=ALU.add),
                 reads=['CORR', pk(ypage)], writes=[pk(ypage)])
            P.op('act', lambda e: e.activation(out=HALO[:, l, m, :], in_=PS[:, b, 510:512], func=AF.Copy),
                 reads=[('ps', b)], writes=[hkey])

        cvi = 0
        for i in range(11):
            sl = next_w()
            for q in range(2):
                m = 2 * i + q
                ya, yv, sa = P_CV + 3 * (cvi % 2), P_CV + 3 * (cvi % 2) + 1, P_CV + 3 * (cvi % 2) + 2
                cvi += 1
                ba = nb()
                for kc in range(8):
                    P.op('pe', lambda e, kc=kc, ba=ba, q=q, sl=sl: e.matmul(
                        PS[:, ba, :], lhsT=RING[:, sl, kc, q * 128:(q + 1) * 128], rhs=H[:, kc, :],
                        start=(kc == 0), stop=(kc == 7)),
                        reads=[('W', sl), ('H', kc)], writes=[('ps', ba)])
                bv = nb()
                for kc in range(8):
                    P.op('pe', lambda e, kc=kc, bv=bv, q=q, sl=sl: e.matmul(
                        PS[:, bv, :], lhsT=RING[:, sl, kc, 256 + q * 128:256 + (q + 1) * 128], rhs=H[:, kc, :],
                        start=(kc == 0), stop=(kc == 7)),
                        reads=[('W', sl), ('H', kc)], writes=[('ps', bv)])
                conv(ba, m, ya)
                conv(bv, NFF + m, yv)
                P.op('act', lambda e, ya=ya, sa=sa: e.activation(out=POOL[:, sa, :], in_=POOL[:, ya, :], func=AF.Silu),
                     reads=[pk(ya)], writes=[pk(sa)])
                P.op('pool', lambda e, sa=sa, yv=yv, m=m: e.tensor_tensor(out=b16(P_G, m), in0=POOL[:, sa, :], in1=POOL[:, yv, :], op=ALU.mult),
                     reads=[pk(sa), pk(yv)], writes=[pk(P_G + m // 2)])
            release_w()

        for hf in range(2):
            slots = [next_w(), next_w(), next_w()]
            for n4 in range(4):
                n = hf * 4 + n4
                b = nb()
                for kf in range(NFF):
                    sl = slots[kf // 8]
                    P.op('pe', lambda e, kf=kf, b=b, sl=sl, n4=n4: e.matmul(
                        PS[:, b, :], lhsT=RING[:, sl, kf % 8, n4 * 128:(n4 + 1) * 128], rhs=b16(P_G, kf),
                        start=(kf == 0), stop=(kf == NFF - 1)),
                        reads=[('W', sl), pk(P_G + kf // 2)], writes=[('ps', b)])
                P.op('dve', lambda e, n=n, b=b: e.scalar_tensor_tensor(
                    out=X[:, n, :], in0=PS[:, b, :], scalar=MOD[:, l, 5 * 8 + n, s:s + 1], in1=X[:, n, :], op0=ALU.mult, op1=ALU.add),
                    reads=[('ps', b), ('X', n), 'MOD'], writes=[('X', n)])
            release_w()
            release_w()
            release_w()

    XT = POOL[:, P_XT:P_XT + 8, :].rearrange("p (b a) n -> p b (a n)", a=2)
    for s in range(n_seq):
        for t in range(n_tiles):
            t0 = t * T
            for blk in range(NBLK):
                P.op('sp', lambda e, blk=blk, s=s, t0=t0: e.dma_start(out=XT[:, blk, :], in_=x_d[s, t0 + blk * 128:t0 + (blk + 1) * 128, :]),
                     writes=pks(P_XT + 2 * blk, 2), dma='xin%d' % blk)
            for dc in range(8):
                b = nb()
                for blk in range(NBLK):
                    P.op('pe', lambda e, b=b, blk=blk, dc=dc: e.transpose(
                        out=PS[:, b, blk * 128:(blk + 1) * 128], in_=XT[:, blk, dc * 128:(dc + 1) * 128], identity=IDF[:]),
                        reads=pks(P_XT + 2 * blk, 2) + ['IDF'], writes=[('ps', b)])
                P.op('act', lambda e, b=b, dc=dc: e.activation(out=X[:, dc, :], in_=PS[:, b, :], func=AF.Copy),
                     reads=[('ps', b)], writes=[('X', dc)])
            for l in layers:
                layer_step(l, s, t, t == 0)
            src_is_X = True
            if final_norm:
                for dc in range(8):
                    P.op('act', lambda e, dc=dc: e.activation(out=b16(P_SQ, dc), in_=X[:, dc, :], func=AF.Square),
                         reads=[('X', dc)], writes=[pk(P_SQ + dc // 2)])
                b = nb()
                for dc in range(8):
                    P.op('pe', lambda e, dc=dc, b=b: e.matmul(PS[:, b, :], lhsT=ONESB[:], rhs=b16(P_SQ, dc),
                                                             start=(dc == 0), stop=(dc == 7)),
                         reads=[pk(P_SQ + dc // 2), 'ONESB'], writes=[('ps', b)])
                P.op('act', lambda e, b=b: e.activation(out=POOL[:, P_LNV, :], in_=PS[:, b, :], func=AF.Ln, scale=1.0 / D, bias=eps_ap),
                     reads=[('ps', b), 'CONST'], writes=[pk(P_LNV)])
                P.op('act', lambda e: e.activation(out=POOL[:, P_RSTD, :], in_=POOL[:, P_LNV, :], func=AF.Exp, scale=-0.5),
                     reads=[pk(P_LNV)], writes=[pk(P_RSTD)])
                for dc in range(8):
                    P.op('dve', lambda e, dc=dc: e.scalar_tensor_tensor(
                        out=X[:, dc, :], in0=X[:, dc, :], scalar=LNF[:, dc:dc + 1], in1=POOL[:, P_RSTD, :], op0=ALU.mult, op1=ALU.mult),
                        reads=[('X', dc), pk(P_RSTD), 'LNF'], writes=[('X', dc)])
            for blk in range(NBLK):
                for half in range(2):
                    b = nb()
                    for d4 in range(4):
                        dc = half * 4 + d4
                        P.op('pe', lambda e, b=b, blk=blk, dc=dc, d4=d4: e.transpose(
                            out=PS[:, b, d4 * 128:(d4 + 1) * 128], in_=X[:, dc, blk * 128:(blk + 1) * 128], identity=IDF[:]),
                            reads=[('X', dc), 'IDF'], writes=[('ps', b)])
                    P.op('act', lambda e, b=b, blk=blk, half=half: e.activation(
                        out=XT[:, blk, half * 512:(half + 1) * 512], in_=PS[:, b, :], func=AF.Copy),
                        reads=[('ps', b)], writes=[pk(P_XT + 2 * blk + half)])
                P.op('sp', lambda e, blk=blk, s=s, t0=t0: e.dma_start(out=y_d[s, t0 + blk * 128:t0 + (blk + 1) * 128, :], in_=XT[:, blk, :]),
                     reads=pks(P_XT + 2 * blk, 2), writes=[('yout', blk)], dma='yout%d' % blk)
    P.wait_all('sp', [('yout', blk) for blk in range(NBLK)])
    P.emit()
    es.close()
    return nc


def _consts():
    ident = np.eye(128, dtype=np.float32)
    p = np.arange(128)[:, None]
    col = np.arange(512)[None, :]
    c = col % 128
    cmask = ((p // 64 == c // 64) & (p % 64 <= c % 64)).astype(np.float32) * np.float32(math.exp(QSHIFT))
    rmask = np.broadcast_to((col % 64 != 0).astype(np.float32), (128, 512)).copy()
    return ident, cmask, rmask


WEIGHT_NAMES = ["ln1_g", "ln2_g", "w_ada", "b_ada", "w_in", "lb_params", "w_gk", "b_gk", "gn_a", "gn_b",
                "w_out", "w_up", "conv_w", "conv_b", "w_down", "lnf_g"]


def run_launch(x, c, weights, layers, final_norm, n_cores):
    B, S_, _ = x.shape
    n_seq = B // n_cores
    nc = build_program(n_seq, S_, layers, True, final_norm)
    ident, cmask, rmask = _consts()
    in_maps = []
    for i in range(n_cores):
        m = {"x": np.ascontiguousarray(x[i * n_seq:(i + 1) * n_seq]),
             "c": np.ascontiguousarray(c[i * n_seq:(i + 1) * n_seq]),
             "ident": ident, "cmask": cmask, "rmask": rmask}
        for k in WEIGHT_NAMES:
            m[k] = weights[k]
        in_maps.append(m)
    res = run_bass_kernel_spmd(nc, in_maps, core_ids=list(range(n_cores)))
    return np.concatenate([r["y"] for r in res.results], axis=0)


FUSED = True


def kernel(**inputs):
    x = np.ascontiguousarray(np.asarray(inputs["x"], dtype=np.float32))
    c = np.ascontiguousarray(np.asarray(inputs["c"], dtype=np.float32))
    weights = {k: np.ascontiguousarray(np.asarray(inputs[k], dtype=np.float32)) for k in WEIGHT_NAMES}
    if FUSED:
        return run_launch(x, c, weights, [0, 1, 2, 3], True, 8)
    for l in range(4):
        x = run_launch(x, c, weights, [l], l == 3, 8)
    return x
```
